# Optimizing a Trainium2 kernel written in Bass

```python
import jax
import jax.numpy as jnp
from jax import lax
import numpy as np

D_MODEL = 1024
BATCH = 4
SEQ = 8192
DEPTH = 4

GRID_W = 64
CTX_LEN = 256
N_MIXERS = 2
N_MOD = 6
EPS = 1e-6
FNET_GROUPS = 4
FNET_GROUP_DIM = D_MODEL // FNET_GROUPS
HEAD_DIM = 64
N_HEADS = D_MODEL // HEAD_DIM
N_KV_HEADS = 4
GROUP = N_HEADS // N_KV_HEADS
QKV_DIM = (N_HEADS + 2 * N_KV_HEADS) * HEAD_DIM
WINDOW = 128
BLOCK = 128
ROPE_THETA = 10000.0
ROT_FREQS = HEAD_DIM // 4
D_FF = D_MODEL * 7 // 2
N_EXPERTS = 8
TOP_K = 2
MOE_BLOCK = 256

kernel_name = "hybrid_fnet_swa_moe_dit"


def rms_norm(x, g):
    xf = x.astype(jnp.float32)
    return (xf * lax.rsqrt(jnp.mean(xf * xf, axis=-1, keepdims=True) + EPS)).astype(x.dtype) * g


def modulate(h, shift, scale):
    return h * (1 + scale) + shift


def swiglu(h, w_gate, w_up, w_down):
    return (jax.nn.silu(h @ w_gate) * (h @ w_up)) @ w_down


def moe_swiglu(h, w_router, w_gate, w_up, w_down):
    T, D = h.shape
    logits = (h @ w_router).astype(jnp.float32)
    top_logit, top_idx = lax.top_k(logits, TOP_K)
    gates = jax.nn.softmax(top_logit, axis=-1)
    flat_e = top_idx.reshape(-1)
    flat_g = gates.reshape(-1)
    n_pairs = T * TOP_K
    order = jnp.argsort(flat_e)
    sorted_e = flat_e[order]
    sorted_tok = (order // TOP_K).astype(jnp.int32)
    counts = jnp.bincount(flat_e, length=N_EXPERTS)
    padded = (counts + MOE_BLOCK - 1) // MOE_BLOCK * MOE_BLOCK
    pad_end = jnp.cumsum(padded)
    pad_start = pad_end - padded
    start = jnp.cumsum(counts) - counts
    dest = pad_start[sorted_e] + jnp.arange(n_pairs) - start[sorted_e]
    n_blocks = -(-n_pairs // MOE_BLOCK) + N_EXPERTS
    cap = n_blocks * MOE_BLOCK
    slot_tok = jnp.zeros((cap,), jnp.int32).at[dest].set(sorted_tok)
    slot_w = jnp.zeros((cap,), jnp.float32).at[dest].set(flat_g[order])
    block_expert = jnp.minimum(
        jnp.searchsorted(pad_end, jnp.arange(n_blocks) * MOE_BLOCK, side='right'), N_EXPERTS - 1)
    xs = h[slot_tok].reshape(n_blocks, MOE_BLOCK, D)

    def expert_group(args):
        xb, e = args
        return swiglu(xb, w_gate[e], w_up[e], w_down[e])

    ys = lax.map(expert_group, (xs, block_expert)).reshape(cap, D)
    return jnp.zeros_like(h).at[slot_tok].add(ys * slot_w[:, None].astype(h.dtype))


def fourier_mix(h, w_out):
    B, N, _ = h.shape
    hg = h.astype(jnp.float32).reshape(B, N, FNET_GROUPS, FNET_GROUP_DIM)
    mixed = jnp.fft.fft2(hg, axes=(1, 3), norm='ortho').real
    return mixed.reshape(B, N, D_MODEL).astype(h.dtype) @ w_out


def axial_rope_tables(n_rows):
    row = jnp.repeat(jnp.arange(n_rows, dtype=jnp.float32), GRID_W)
    col = jnp.tile(jnp.arange(GRID_W, dtype=jnp.float32), n_rows)
    inv_freq = ROPE_THETA ** (-jnp.arange(ROT_FREQS, dtype=jnp.float32) / ROT_FREQS)
    ang = jnp.stack([row[:, None] * inv_freq, col[:, None] * inv_freq], axis=1)
    return jnp.cos(ang), jnp.sin(ang)


def apply_rope(x, cos, sin):
    xr = x.reshape(x.shape[:3] + (2, 2, ROT_FREQS))
    x1 = xr[..., 0, :]
    x2 = xr[..., 1, :]
    c = cos[None, :, None].astype(x.dtype)
    s = sin[None, :, None].astype(x.dtype)
    return jnp.stack([x1 * c - x2 * s, x2 * c + x1 * s], axis=-2).reshape(x.shape)


def window_gqa_attention(h_lat, h_ctx, w_qkv, w_o, sinks, cos, sin, ctx_out):
    B, S, _ = h_lat.shape
    L = h_ctx.shape[1]
    nb = S // BLOCK
    scale = HEAD_DIM ** -0.5

    def project(h):
        n = h.shape[1]
        q, k, v = jnp.split(h @ w_qkv, [N_HEADS * HEAD_DIM, (N_HEADS + N_KV_HEADS) * HEAD_DIM], axis=-1)
        return (q.reshape(B, n, N_HEADS, HEAD_DIM), k.reshape(B, n, N_KV_HEADS, HEAD_DIM),
                v.reshape(B, n, N_KV_HEADS, HEAD_DIM))

    q_l, k_l, v_l = project(h_lat)
    q_l = apply_rope(q_l, cos, sin).reshape(B, S, N_KV_HEADS, GROUP, HEAD_DIM)
    k_l = apply_rope(k_l, cos, sin)
    q_c, k_c, v_c = project(h_ctx)
    sink = sinks.astype(jnp.float32).reshape(N_KV_HEADS, GROUP)[None, :, :, None, None]

    def sink_softmax(logits):
        b, kv, g, nq, _ = logits.shape
        sink_col = jnp.broadcast_to(sink, (b, kv, g, nq, 1))
        return jax.nn.softmax(jnp.concatenate([logits, sink_col], axis=-1), axis=-1)[..., :-1]

    def band(t):
        tp = jnp.pad(t, ((0, 0), (BLOCK, BLOCK), (0, 0), (0, 0))).reshape(B, nb + 2, BLOCK, N_KV_HEADS, HEAD_DIM)
        w = jnp.concatenate([tp[:, :-2], tp[:, 1:-1], tp[:, 2:]], axis=2)
        return w.transpose(1, 0, 2, 3, 4)

    q_idx = jnp.arange(BLOCK)[:, None]
    k_idx = jnp.arange(3 * BLOCK)[None, :]
    in_window = jnp.abs(k_idx - BLOCK - q_idx) <= WINDOW
    key_pos = jnp.arange(nb)[:, None] * BLOCK - BLOCK + jnp.arange(3 * BLOCK)[None, :]
    in_range = (key_pos >= 0) & (key_pos < S)
    mask = jnp.concatenate([in_window[None] & in_range[:, None, :],
                            jnp.ones((nb, BLOCK, L), dtype=bool)], axis=-1)

    def latent_block(args):
        qb, kb, vb, mb = args
        k_all = jnp.concatenate([kb, k_c], axis=1)
        v_all = jnp.concatenate([vb, v_c], axis=1)
        logits = jnp.einsum('bqkgd,bjkd->bkgqj', qb, k_all).astype(jnp.float32) * scale
        logits = jnp.where(mb, logits, -jnp.inf)
        p = sink_softmax(logits).astype(vb.dtype)
        return jnp.einsum('bkgqj,bjkd->bqkgd', p, v_all)

    q_blocks = q_l.reshape(B, nb, BLOCK, N_KV_HEADS, GROUP, HEAD_DIM).transpose(1, 0, 2, 3, 4, 5)
    o_l = lax.map(latent_block, (q_blocks, band(k_l), band(v_l), mask))
    o_l = o_l.transpose(1, 0, 2, 3, 4, 5).reshape(B, S, N_HEADS * HEAD_DIM) @ w_o
    if not ctx_out:
        return o_l, None
    q_c = q_c.reshape(B, L, N_KV_HEADS, GROUP, HEAD_DIM)
    logits_c = jnp.einsum('bqkgd,bjkd->bkgqj', q_c, k_c).astype(jnp.float32) * scale
    p_c = sink_softmax(logits_c).astype(v_c.dtype)
    o_c = jnp.einsum('bkgqj,bjkd->bqkgd', p_c, v_c).reshape(B, L, N_HEADS * HEAD_DIM) @ w_o
    return o_l, o_c


def setup_inputs(seed: int = 0) -> dict:
    key = jax.random.key(seed)
    ks = jax.random.split(key, 20)
    n_even = (DEPTH + 1) // 2
    n_odd = DEPTH // 2
    f32 = jnp.float32

    def normal(k, shape):
        return jax.random.normal(k, shape, f32)

    def dense(k, shape, fan_in):
        return normal(k, shape) * fan_in ** -0.5

    return {
        'x': normal(ks[0], (BATCH, SEQ, D_MODEL)),
        'c': normal(ks[1], (BATCH, D_MODEL)),
        'ctx': normal(ks[2], (BATCH, CTX_LEN, D_MODEL)),
        'c_ctx': normal(ks[3], (D_MODEL,)),
        'ada_w': dense(ks[4], (DEPTH, D_MODEL, N_MOD * D_MODEL), D_MODEL) * 0.5,
        'ada_b': 0.02 * normal(ks[5], (DEPTH, N_MOD * D_MODEL)),
        'norm_mix_g': 1.0 + 0.05 * normal(ks[6], (DEPTH, D_MODEL)),
        'norm_ffn_g': 1.0 + 0.05 * normal(ks[7], (DEPTH, D_MODEL)),
        'fnet_w_out': dense(ks[8], (n_even, D_MODEL, D_MODEL), D_MODEL),
        'attn_w_qkv': dense(ks[9], (n_odd, D_MODEL, QKV_DIM), D_MODEL),
        'attn_w_o': dense(ks[10], (n_odd, N_HEADS * HEAD_DIM, D_MODEL), N_HEADS * HEAD_DIM),
        'attn_sinks': normal(ks[11], (n_odd, N_HEADS)),
        'ffn_w_gate': dense(ks[12], (n_even, D_MODEL, D_FF), D_MODEL),
        'ffn_w_up': dense(ks[13], (n_even, D_MODEL, D_FF), D_MODEL),
        'ffn_w_down': dense(ks[14], (n_even, D_FF, D_MODEL), D_FF),
        'moe_w_router': dense(ks[15], (n_odd, D_MODEL, N_EXPERTS), D_MODEL),
        'moe_w_gate': dense(ks[16], (n_odd, N_EXPERTS, D_MODEL, D_FF), D_MODEL),
        'moe_w_up': dense(ks[17], (n_odd, N_EXPERTS, D_MODEL, D_FF), D_MODEL),
        'moe_w_down': dense(ks[18], (n_odd, N_EXPERTS, D_FF, D_MODEL), D_FF),
        'final_norm_g': 1.0 + 0.05 * normal(ks[19], (D_MODEL,)),
    }


def reference(x, c, ctx, c_ctx, ada_w, ada_b, norm_mix_g, norm_ffn_g, fnet_w_out, attn_w_qkv, attn_w_o,
              attn_sinks, ffn_w_gate, ffn_w_up, ffn_w_down, moe_w_router, moe_w_gate, moe_w_up, moe_w_down,
              final_norm_g):
    B, S, D = x.shape
    L = ctx.shape[1]
    rows = S // GRID_W
    cos, sin = axial_rope_tables(rows)
    silu_c = jax.nn.silu(c)
    silu_cc = jax.nn.silu(c_ctx)
    x_lat, x_ctx = x, ctx
    for i in range(DEPTH):
        j = i // 2
        last = i == DEPTH - 1
        is_attn = i % N_MIXERS == 1
        sh_l, sc_l, g_l, shf_l, scf_l, gf_l = jnp.split(silu_c @ ada_w[i] + ada_b[i], N_MOD, axis=-1)
        sh_c, sc_c, g_c, shf_c, scf_c, gf_c = jnp.split(silu_cc @ ada_w[i] + ada_b[i], N_MOD, axis=-1)
        h_lat = modulate(rms_norm(x_lat, norm_mix_g[i]), sh_l[:, None], sc_l[:, None])
        if is_attn:
            h_ctx = modulate(rms_norm(x_ctx, norm_mix_g[i]), sh_c, sc_c)
            y_lat, y_ctx = window_gqa_attention(h_lat, h_ctx, attn_w_qkv[j], attn_w_o[j], attn_sinks[j],
                                                cos, sin, not last)
        else:
            y_lat = fourier_mix(h_lat, fnet_w_out[j])
            if not last:
                y_ctx = fourier_mix(modulate(rms_norm(x_ctx, norm_mix_g[i]), sh_c, sc_c), fnet_w_out[j])
        x_lat = x_lat + g_l[:, None] * y_lat
        if not last:
            x_ctx = x_ctx + g_c * y_ctx
        tokens = modulate(rms_norm(x_lat, norm_ffn_g[i]), shf_l[:, None], scf_l[:, None]).reshape(B * S, D)
        if not last:
            hf_ctx = modulate(rms_norm(x_ctx, norm_ffn_g[i]), shf_c, scf_c).reshape(B * L, D)
            tokens = jnp.concatenate([tokens, hf_ctx], axis=0)
        if i % 2 == 0:
            y = swiglu(tokens, ffn_w_gate[j], ffn_w_up[j], ffn_w_down[j])
        else:
            y = moe_swiglu(tokens, moe_w_router[j], moe_w_gate[j], moe_w_up[j], moe_w_down[j])
        x_lat = x_lat + gf_l[:, None] * y[:B * S].reshape(B, S, D)
        if not last:
            x_ctx = x_ctx + gf_c * y[B * S:].reshape(B, L, D)
    return rms_norm(x_lat, final_norm_g)
```

```python
import numpy as np
import contextlib
import concourse.bass as bass
import concourse.mybir as mybir
from concourse.bass_utils import run_bass_kernel_spmd

F32 = mybir.dt.float32
BF16 = mybir.dt.bfloat16
I32 = mybir.dt.int32
U32 = mybir.dt.uint32
AF = mybir.ActivationFunctionType
ALU = mybir.AluOpType
AX = mybir.AxisListType

D = 1024
S = 8192
LCTX = 256
DFF = 3584
NF = 28
EPS = 1e-6


class Tr:
    def __init__(self, nc, es):
        self.nc = nc
        self.es = es
        self.eng = {'pe': nc.tensor, 'act': nc.scalar, 'dve': nc.vector, 'pool': nc.gpsimd, 'sp': nc.sync}
        self.esem = {}
        self.ecnt = {}
        self.known = {k: {} for k in self.eng}
        self.state = {}
        self.dsem = {}
        self.dtot = {}
        for k in ['pe', 'act', 'dve', 'pool']:
            self.esem[k] = es.enter_context(nc.semaphore("es_" + k))
            self.ecnt[k] = 0
        self.nwaits = 0
        self.nops = 0
        self.uq_free = []
        self.uq_used = []
        self.uq_n = 0

    def dma_sem(self, name):
        if name not in self.dsem:
            s = self.es.enter_context(self.nc.semaphore("ds_" + name))
            self.dsem[name] = [s, 0]
            self.dtot[id(s)] = self.dsem[name]
        return self.dsem[name]

    def _need(self, e, ev):
        sem, val, src = ev
        if src == e and e == 'pe':
            return
        k = self.known[e]
        if src == 'dma':
            val = max(val, self.dtot[id(sem)][1])
        if k.get(id(sem), 0) >= val:
            return
        self.eng[e].wait_ge(sem, val)
        self.nwaits += 1
        k[id(sem)] = val

    def op(self, e, fn, reads=(), writes=(), dsem=None):
        for key in reads:
            st = self.state.get(key)
            if st:
                for ev in st['w']:
                    self._need(e, ev)
        for key in writes:
            st = self.state.get(key)
            if st:
                for ev in st['w']:
                    self._need(e, ev)
                for ev in st['r']:
                    self._need(e, ev)
        ins = fn(self.eng[e])
        self.nops += 1
        if dsem is not None:
            if dsem in ('c0', 'c1', 'uniq'):
                if not self.uq_free:
                    self.uq_n += 1
                    self.uq_free.append('uq%d' % self.uq_n)
                dsem = self.uq_free.pop()
                self.uq_used.append(dsem)
            d = self.dma_sem(dsem)
            d[1] += 16
            ins.then_inc(d[0], 16)
            ev = (d[0], d[1], 'dma')
        else:
            self.ecnt[e] += 1
            ins.then_inc(self.esem[e], 1)
            ev = (self.esem[e], self.ecnt[e], e)
        for key in reads:
            st = self.state.setdefault(key, {'w': [], 'r': []})
            st['r'].append(ev)
            if len(st['r']) > 64:
                best = {}
                for s_, v_, src_ in st['r']:
                    if id(s_) not in best or best[id(s_)][1] < v_:
                        best[id(s_)] = (s_, v_, src_)
                st['r'] = list(best.values())
        for key in writes:
            self.state[key] = {'w': [ev], 'r': []}
        return ev

    def barrier(self, engines=('pe', 'act', 'dve', 'pool', 'sp')):
        for e in engines:
            for k in ['pe', 'act', 'dve', 'pool']:
                if self.ecnt[k] > 0:
                    self._need(e, (self.esem[k], self.ecnt[k], k if k != 'pe' else 'x'))
            for name, (s, c) in self.dsem.items():
                if c > 0:
                    self._need(e, (s, c, 'dma'))
        self.state = {}
        self.uq_free.extend(self.uq_used)
        self.uq_used = []


class Ring:
    def __init__(self, n):
        self.n = n
        self.i = -1

    def next(self):
        self.i = (self.i + 1) % self.n
        return self.i


_PFX = ['']


def setup_common(nc, T, es, sb, ps_banks, ident_d):
    idf = sb("idf", [128, 128], F32)
    idb = sb("idb", [128, 128], BF16)
    ones = sb("ones", [128, 128], F32)
    T.op('sp', lambda e: e.dma_start(out=idf[:], in_=ident_d), writes=['idf'], dsem='c0')
    T.op('dve', lambda e: e.tensor_copy(out=idb[:], in_=idf[:]), reads=['idf'], writes=['idb'])
    T.op('pool', lambda e: e.memset(ones[:], 1.0), writes=['ones'])
    return idf, idb, ones


def compute_mod(nc, T, sb_tmp, ps, cvec, ada_w, ada_b, ones, mods, gains, cache=None, load=False):
    if load:
        for (which, idx), tile in mods.items():
            T.op('sp', lambda e: e.dma_start(out=tile[:], in_=cache[which * 6 + idx]), writes=[('mod', which, idx)], dsem='uniq')
        T.barrier()
        return
    with contextlib.ExitStack() as es2:
        def sb(name, shape, dt):
            return es2.enter_context(nc.sbuf_tensor(_PFX[0] + name, shape, dt))
        cv = sb("cv", [128, 16], F32)
        sc = sb("scv", [128, 16], F32)
        screp = sb("screp", [128, 16, 128], F32)
        brow = sb("brow", [1, 6 * D], F32)
        grow = sb("grow", [1, 2 * D], F32)
        aw = [sb("aw%d" % i, [128, 8, 512], F32) for i in range(2)]
        T.op('sp', lambda e: e.dma_start(out=cv[:], in_=cvec), writes=['cv'], dsem='c0')
        T.op('sp', lambda e: e.dma_start(out=brow[:], in_=ada_b), writes=['brow'], dsem='c0')
        for gi, (gd, _) in enumerate(gains):
            T.op('sp', lambda e: e.dma_start(out=grow[:, gi * D:(gi + 1) * D], in_=gd), writes=[('grow', gi)], dsem='c0')
        T.op('act', lambda e: e.activation(out=sc[:], in_=cv[:], func=AF.Silu), reads=['cv'], writes=['sc'])
        for j in range(16):
            T.op('dve', lambda e: e.tensor_scalar(out=screp[:, j, :], in0=ones[:], scalar1=sc[:, j:j + 1], scalar2=None, op0=ALU.mult),
                 reads=['sc', 'ones'], writes=[('screp', j)])
        awv = ada_w.rearrange("(c p) f -> p c f", p=128)
        for n in range(12):
            a = aw[n % 2]
            ak = ('aw', n % 2)
            T.op('sp', lambda e: e.dma_start(out=a[:], in_=awv[:, :, n * 512:(n + 1) * 512]), writes=[ak], dsem='aw%d' % (n % 2))
            for which in range(2):
                bank = ps[which]
                bk = ('ps', which)
                for kc in range(8):
                    T.op('pe', lambda e: e.matmul(bank[:], lhsT=screp[:, which * 8 + kc, :], rhs=a[:, kc, :], start=(kc == 0), stop=False),
                         reads=[('screp', which * 8 + kc), ak], writes=[bk])
                T.op('pe', lambda e: e.matmul(bank[:], lhsT=ones[0:1, :], rhs=brow[0:1, n * 512:(n + 1) * 512], start=False, stop=True),
                     reads=['ones', 'brow'], writes=[bk])
                idx = n // 2
                dst = mods[(which, idx)]
                half = n % 2
                T.op('dve' if which == 0 else 'act',
                     (lambda e: e.tensor_copy(out=dst[:, half * 512:(half + 1) * 512], in_=bank[:])) if which == 0 else
                     (lambda e: e.activation(out=dst[:, half * 512:(half + 1) * 512], in_=bank[:], func=AF.Copy)),
                     reads=[bk], writes=[('mod', which, idx)])
        for gi, (gd, lst) in enumerate(gains):
            for half in range(2):
                bank = ps[2 + half]
                bk = ('ps', 2 + half)
                T.op('pe', lambda e: e.matmul(bank[:], lhsT=ones[0:1, :], rhs=grow[0:1, gi * D + half * 512: gi * D + (half + 1) * 512], start=True, stop=True),
                     reads=['ones', ('grow', gi)], writes=[bk])
                for (which, idx) in lst:
                    dst = mods[(which, idx)]
                    T.op('dve', lambda e: e.scalar_tensor_tensor(out=dst[:, half * 512:(half + 1) * 512], in0=dst[:, half * 512:(half + 1) * 512],
                                                                 scalar=1.0, in1=bank[:], op0=ALU.add, op1=ALU.mult),
                         reads=[bk, ('mod', which, idx)], writes=[('mod', which, idx)])
        if cache is not None:
            for (which, idx), tile in mods.items():
                T.op('sp', lambda e: e.dma_start(out=cache[which * 6 + idx], in_=tile[:]), reads=[('mod', which, idx)], writes=[('modc', which, idx)], dsem='modw')
        T.barrier()


class NormCtx:
    def __init__(self, nc, T, sb, idb, pfx="", nbuf=2):
        self.nc = nc
        self.T = T
        self.idb = idb
        self.pfx = pfx
        self.junk = sb(pfx + "junk", [128, 1024], BF16)
        self.ss = [sb(pfx + "ss%d" % i, [128, 2], F32) for i in range(nbuf)]
        self.h1 = [sb(pfx + "h1_%d" % i, [128, 1024], F32) for i in range(nbuf)]
        self.hb = [sb(pfx + "hb_%d" % i, [128, 1024], BF16) for i in range(nbuf)]
        self.r = Ring(nbuf)
        self.nhalf = sb(pfx + "nhalf", [128, 1], F32)
        T.op('pool', lambda e: e.memset(self.nhalf[:], -0.5), writes=[pfx + 'nhalf'])

    def run(self, xt, xkey, GS, gskey, SH, shkey, psT, pskey, out_ap, outkey, evac_eng='act'):
        i = self.pre(xt, xkey, GS, gskey, SH, shkey)
        self.post(i, psT, pskey, out_ap, outkey, evac_eng)

    def pre(self, xt, xkey, GS, gskey, SH, shkey):
        T = self.T
        i = self.r.next()
        p = self.pfx
        ss, h1, hb = self.ss[i], self.h1[i], self.hb[i]
        T.op('act', lambda e: e.activation(out=self.junk[:], in_=xt, func=AF.Square, accum_out=ss[:, 0:1]), reads=[xkey], writes=[(p + 'ss', i)])
        T.op('dve', lambda e: e.tensor_scalar(out=ss[:, 1:2], in0=ss[:, 0:1], scalar1=1.0 / D, scalar2=EPS, op0=ALU.mult, op1=ALU.add),
             reads=[(p + 'ss', i)], writes=[(p + 'ss1', i)])
        T.op('pool', lambda e: e.tensor_tensor(out=ss[:, 1:2], in0=ss[:, 1:2], in1=self.nhalf[:], op=ALU.pow),
             reads=[(p + 'ss1', i), p + 'nhalf'], writes=[(p + 'ss1', i)])
        T.op('dve', lambda e: e.scalar_tensor_tensor(out=h1[:], in0=xt, scalar=ss[:, 1:2], in1=GS, op0=ALU.mult, op1=ALU.mult),
             reads=[xkey, (p + 'ss1', i), gskey], writes=[(p + 'h1', i)])
        T.op('pool', lambda e: e.tensor_tensor(out=hb[:], in0=h1[:], in1=SH, op=ALU.add), reads=[(p + 'h1', i), shkey], writes=[(p + 'hb', i)])
        return i

    def post(self, i, psT, pskey, out_ap, outkey, evac_eng='act'):
        T = self.T
        p = self.pfx
        hb = self.hb[i]
        pT = psT[:].bitcast(BF16).rearrange("p (c t) -> p c t", c=8)
        for c in range(8):
            T.op('pe', lambda e: e.transpose(out=pT[:, c, :], in_=hb[:, c * 128:(c + 1) * 128], identity=self.idb[:]),
                 reads=[(p + 'hb', i), 'idb'], writes=[pskey])
        if evac_eng == 'act':
            T.op('act', lambda e: e.activation(out=out_ap, in_=pT, func=AF.Copy), reads=[pskey], writes=[outkey])
        else:
            T.op('dve', lambda e: e.tensor_copy(out=out_ap, in_=pT), reads=[pskey], writes=[outkey])


def emit_fnet(nc, T, ps, common, W, A, SC, s, first_pass, lat_tiles=None):
    debug_stop = None
    idf, idb, ones = common
    xfull = A['xin']; xc = A['cin']; xown = xfull[s * 4096:(s + 1) * 4096, :]
    yown = A['yout'][s * 4096:(s + 1) * 4096, :]; yc = A['cout']
    cvec = W['cvec']; ada_w = W['ada_w']; ada_b = W['ada_b']; nmg = W['nmg']; nfg = W['nfg']
    w_out = W['w_out']; wg = W['wg']; wu = W['wu']; wd = W['wd']
    cs256 = W['cs256']; twc = W['twc']; tws = W['tws']; w64 = W['w64_%d' % s]
    ABd = SC['ABd']; Gd = SC['Gd']; X1d = SC['X1d']; wgb = SC['wgb']; wub = SC['wub']; wdb = SC['wdb']; woutb = SC['woutb']
    NA = 1 if first_pass else 0
    if lat_tiles is None:
        lat_tiles = list(range(32))

    with contextlib.ExitStack() as es:
        def sbg(name, shape, dtp):
            return es.enter_context(nc.sbuf_tensor(_PFX[0] + name, shape, dtp))
        if first_pass:
          T.op('pool', lambda e: e.dma_start(out=wgb.rearrange("k (a b) -> (k a) b", b=1792), in_=wg.rearrange("k (a b) -> (k a) b", b=1792)), writes=['wgb'], dsem='wconv')
          T.op('pool', lambda e: e.dma_start(out=wub.rearrange("k (a b) -> (k a) b", b=1792), in_=wu.rearrange("k (a b) -> (k a) b", b=1792)), writes=['wub'], dsem='wconv')
          T.op('pool', lambda e: e.dma_start(out=wdb, in_=wd), writes=['wdb'], dsem='wconv')
          T.op('pool', lambda e: e.dma_start(out=woutb, in_=w_out), writes=['woutb'], dsem='wconv')
        mods = {}
        for which in range(2):
            for idx in (3, 4, 5):
                mods[(which, idx)] = sbg("mod%d_%d" % (which, idx), [128, D], F32)
        with contextlib.ExitStack() as es_mix:
            def sbm(name, shape, dtp):
                return es_mix.enter_context(nc.sbuf_tensor(_PFX[0] + name, shape, dtp))
            for which in range(2):
                for idx in (0, 1, 2):
                    mods[(which, idx)] = sbm("mod%d_%d" % (which, idx), [128, D], F32)
            compute_mod(nc, T, None, ps, cvec, ada_w, ada_b, ones, mods,
                        [(nmg, [(0, 1), (1, 1)]), (nfg, [(0, 4), (1, 4)])], cache=SC['modc'], load=(not first_pass))
            YT = sbm("YT", [128, 8, 4096], BF16)
            YTc = sbm("YTc", [128, 8, 256], BF16)
            CS = sbm("CS", [128, 2, 512], BF16)
            T.op('pool', lambda e: e.dma_start(out=CS[:], in_=cs256.rearrange("(k p) f -> p k f", p=128)), writes=['CS'], dsem='c1')

            with contextlib.ExitStack() as esA:
                def sba(name, shape, dtp):
                    return esA.enter_context(nc.sbuf_tensor(_PFX[0] + name, shape, dtp))
                NC = NormCtx(nc, T, sba, idb, "A", nbuf=4)
                xt = [sba("Axt%d" % i, [128, D], F32) for i in range(4)]
                hT = [sba("AhT%d" % i, [128, 8, 128], BF16) for i in range(3)]
                ABt = [sba("ABt%d" % i, [128, 2048], BF16) for i in range(3)]
                ABc = sba("ABc", [128, 2, 2048], BF16)
                Bnc = sba("Bnc", [128, 2, 1024], BF16)
                NTA = (64 + 2) * NA
                xr = Ring(4); hr = Ring(3); ar = Ring(3); dbank = [0]

                def src_tile(t):
                    return xfull[t * 128:(t + 1) * 128, :] if t < 64 else xc[(t - 64) * 128:(t - 63) * 128, :]
                loaded = {}

                def issue_load(t):
                    if t >= NTA:
                        return
                    i = xr.next()
                    T.op('sp', lambda e: e.dma_start(out=xt[i][:], in_=src_tile(t)), writes=[('Axt', i)], dsem='Axt%d' % i)
                    loaded[t] = i
                issue_load(0); issue_load(1); issue_load(2)
                for t in range(NTA):
                    issue_load(t + 3)
                    i = loaded[t]
                    which = 0 if t < 64 else 1
                    hi = hr.next()
                    NC.run(xt[i][:], ('Axt', i), mods[(which, 1)][:], ('mod', which, 1), mods[(which, 0)][:], ('mod', which, 0),
                           ps[6 + (t % 2)], ('ps', 6 + (t % 2)), hT[hi][:], ('AhT', hi), evac_eng='act')
                    ai = ar.next()
                    for g in range(4):
                        gb = dbank[0] % 6; dbank[0] += 1
                        for kk in range(2):
                            T.op('pe', lambda e: e.matmul(ps[gb][:], lhsT=hT[hi][:, 2 * g + kk, :], rhs=CS[:, kk, :], start=(kk == 0), stop=(kk == 1)),
                                 reads=[('AhT', hi), 'CS'], writes=[('ps', gb)])
                        if t < 64:
                            dst = ABt[ai][:, g * 512:(g + 1) * 512]; dk = ('ABt', ai, g)
                        else:
                            dst = ABc[:, t - 64, g * 512:(g + 1) * 512]; dk = ('ABc', t - 64, g)
                        if g % 2 == 0:
                            T.op('dve', lambda e: e.tensor_copy(out=dst, in_=ps[gb][:]), reads=[('ps', gb)], writes=[dk])
                        else:
                            T.op('act', lambda e: e.activation(out=dst, in_=ps[gb][:], func=AF.Copy), reads=[('ps', gb)], writes=[dk])
                    if t < 64:
                        T.op('sp', lambda e: e.dma_start(out=ABd[t * 128:(t + 1) * 128, :], in_=ABt[ai][:]),
                             reads=[('ABt', ai, g) for g in range(4)], writes=[('ABd', t)], dsem='abw')
                for tt in range(2 * NA):
                    T.op('pool', lambda e: e.tensor_scalar(out=Bnc[:, tt, :].rearrange("p (g x) -> p g x", g=4),
                                                           in0=ABc[:, tt, :].rearrange("p (g x) -> p g x", g=4)[:, :, 256:512],
                                                           scalar1=-1.0, scalar2=None, op0=ALU.mult),
                         reads=[('ABc', tt, g) for g in range(4)], writes=[('Bnc', tt)])
                for c in range(8 * NA):
                    g = c // 2
                    bank = ps[c // 2]
                    o = bank[:, (c % 2) * 256:(c % 2) * 256 + 256]
                    n_mm = 0
                    for nt in range(2):
                        T.op('pe', lambda e: e.matmul(o, lhsT=ABc[:, nt, g * 512 + (c % 2) * 128: g * 512 + (c % 2) * 128 + 128], rhs=CS[:, nt, 0:256], start=(nt == 0), stop=False),
                             reads=[('ABc', nt, g), 'CS'], writes=[('ps', c // 2)])
                        T.op('pe', lambda e: e.matmul(o, lhsT=Bnc[:, nt, g * 256 + (c % 2) * 128: g * 256 + (c % 2) * 128 + 128], rhs=CS[:, nt, 256:512], start=False, stop=(nt == 1)),
                             reads=[('Bnc', nt), 'CS'], writes=[('ps', c // 2)])
                    if c % 2 == 1:
                        T.op('act', lambda e: e.activation(out=YTc[:, c - 1:c + 1, :], in_=bank[:].rearrange("p (c k) -> p c k", c=2), func=AF.Identity, scale=1.0 / 256.0),
                             reads=[('ps', c // 2)], writes=[('YTc', c // 2)])
                T.barrier()
            if debug_stop == 'A':
                return nc
            with contextlib.ExitStack() as esS:
                def sbs(name, shape, dtp):
                    return esS.enter_context(nc.sbuf_tensor(_PFX[0] + name, shape, dtp))
                TWC = sbs("TWC", [128, 64, 128], BF16); TWS = sbs("TWS", [128, 64, 128], BF16)
                for q in range(4 * NA):
                    T.op('pool', lambda e: e.dma_start(out=TWC[:, q * 16:(q + 1) * 16, :], in_=twc.rearrange("p (j k) -> p j k", k=128)[:, q * 16:(q + 1) * 16, :]), writes=[('TWC', q)], dsem='c1')
                    T.op('pool', lambda e: e.dma_start(out=TWS[:, q * 16:(q + 1) * 16, :], in_=tws.rearrange("p (j k) -> p j k", k=128)[:, q * 16:(q + 1) * 16, :]), writes=[('TWS', q)], dsem='c1')
                Z = [sbs("Z%d" % i, [128, 4, 2048], BF16) for i in range(2)]
                Bn = [sbs("Bn%d" % i, [128, 1024], BF16) for i in range(2)]
                Gt = [sbs("Gt%d" % i, [128, 2, 1024], BF16) for i in range(2)]
                ABv = ABd.rearrange("(m j) c -> m j c", j=64)
                abkeys = [('ABd', t) for t in range(64)]
                zload = {}

                def issue_z(jg):
                    if jg >= 16 * NA:
                        return
                    i = jg % 2
                    T.op('sp', lambda e: e.dma_start(out=Z[i][:], in_=ABv[:, jg * 4:(jg + 1) * 4, :]), reads=abkeys, writes=[('Z', i)], dsem='Z%d' % i)
                issue_z(0)
                sc1 = 1.0 / np.sqrt(8192.0 * 256.0)
                for jg in range(16 * NA):
                    issue_z(jg + 1)
                    zi = jg % 2
                    for jj in range(4):
                        j = jg * 4 + jj
                        q = j // 16
                        bi = j % 2
                        Zv = Z[zi][:, jj, :].rearrange("p (g x) -> p g x", g=4)
                        A = Zv[:, :, 0:256]; B = Zv[:, :, 256:512]
                        Bnv = Bn[bi][:].rearrange("p (g x) -> p g x", g=4)
                        T.op('act', lambda e: e.activation(out=Bnv, in_=B, func=AF.Identity, scale=-1.0), reads=[('Z', zi)], writes=[('Bn', bi)])
                        for h in range(2):
                            br = ps[(j % 2) * 4 + h * 2]; bkr = ('ps', (j % 2) * 4 + h * 2)
                            bim = ps[(j % 2) * 4 + h * 2 + 1]; bki = ('ps', (j % 2) * 4 + h * 2 + 1)
                            T.op('pe', lambda e: e.matmul(br[:], lhsT=TWC[:, j, :], rhs=A[:, 2 * h:2 * h + 2, :], start=True, stop=False), reads=[('TWC', q), ('Z', zi)], writes=[bkr])
                            T.op('pe', lambda e: e.matmul(br[:], lhsT=TWS[:, j, :], rhs=Bnv[:, 2 * h:2 * h + 2, :], start=False, stop=True), reads=[('TWS', q), ('Bn', bi)], writes=[bkr])
                            T.op('pe', lambda e: e.matmul(bim[:], lhsT=TWS[:, j, :], rhs=A[:, 2 * h:2 * h + 2, :], start=True, stop=False), reads=[('TWS', q), ('Z', zi)], writes=[bki])
                            T.op('pe', lambda e: e.matmul(bim[:], lhsT=TWC[:, j, :], rhs=B[:, 2 * h:2 * h + 2, :], start=False, stop=True), reads=[('TWC', q), ('Z', zi)], writes=[bki])
                            T.op('act', lambda e: e.activation(out=Gt[bi][:, 0, h * 512:(h + 1) * 512], in_=br[:], func=AF.Identity, scale=sc1), reads=[bkr], writes=[('Gt', bi, 0, h)])
                            T.op('dve', lambda e: e.tensor_scalar(out=Gt[bi][:, 1, h * 512:(h + 1) * 512], in0=bim[:], scalar1=sc1, scalar2=None, op0=ALU.mult), reads=[bki], writes=[('Gt', bi, 1, h)])
                        T.op('sp', lambda e: e.dma_start(out=Gd[j], in_=Gt[bi][:]), reads=[('Gt', bi, r, h) for r in range(2) for h in range(2)], writes=[('Gd', j)], dsem='gw')
                T.barrier()
            if debug_stop == 'S1':
                return nc
            with contextlib.ExitStack() as es2:
                def sb2(name, shape, dtp):
                    return es2.enter_context(nc.sbuf_tensor(_PFX[0] + name, shape, dtp))
                W64f = sb2("W64f", [128, 32], F32); W64 = sb2("W64", [128, 32], BF16)
                T.op('sp', lambda e: e.dma_start(out=W64f[:], in_=w64), writes=['W64f'], dsem='c0')
                T.op('dve', lambda e: e.tensor_copy(out=W64[:], in_=W64f[:]), reads=['W64f'], writes=['W64'])
                G2 = [sb2("G2_%d" % i, [128, 4, 1024], BF16) for i in range(3)]
                gkeys = [('Gd', j) for j in range(64)]
                YTv = YT[:].rearrange("p c (a b) -> p c a b", b=128)

                def issue_g(kg):
                    if kg >= 32:
                        return
                    i = kg % 3
                    for ri in range(2):
                        T.op('sp', lambda e: e.dma_start(out=G2[i][ri * 64:(ri + 1) * 64, :, :], in_=Gd[:, kg * 4:(kg + 1) * 4, ri, :]), reads=gkeys, writes=[('G2', i, ri)], dsem='G2_%d' % i)
                issue_g(0); issue_g(1)
                for kg in range(32):
                    issue_g(kg + 2)
                    i = kg % 3
                    for cb in range(2):
                        bank = ps[(kg % 2) * 2 + cb]; bk = ('ps', (kg % 2) * 2 + cb)
                        bv = bank[:].rearrange("p (c b a) -> p c b a", c=4, b=4)
                        for kbi in range(4):
                            for cc in range(4):
                                c = cb * 4 + cc
                                T.op('pe', lambda e: e.matmul(bv[:, cc, kbi, :], lhsT=G2[i][:, kbi, c * 128:(c + 1) * 128], rhs=W64[:], start=True, stop=True),
                                     reads=[('G2', i, 0), ('G2', i, 1), 'W64'], writes=[bk])
                        src = bank[:].rearrange("p (c b a) -> p c a b", c=4, b=4)
                        dst = YTv[:, cb * 4:(cb + 1) * 4, :, kg * 4:(kg + 1) * 4]
                        if cb == 0:
                            T.op('act', lambda e: e.activation(out=dst, in_=src, func=AF.Copy), reads=[bk], writes=[('YT', kg, cb)])
                        else:
                            T.op('dve', lambda e: e.tensor_copy(out=dst, in_=src), reads=[bk], writes=[('YT', kg, cb)])
                T.barrier()
            if debug_stop == 'S2':
                return nc
            with contextlib.ExitStack() as esB:
                def sbb(name, shape, dtp):
                    return esB.enter_context(nc.sbuf_tensor(_PFX[0] + name, shape, dtp))
                Wo = sbb("Wo", [128, 8, 1024], BF16)
                T.op('sp', lambda e: e.dma_start(out=Wo[:], in_=woutb.rearrange("(c p) f -> p c f", p=128)), reads=['woutb'], writes=['Wo'], dsem='c0')
                xt = [sbb("Bxt%d" % i, [128, D], F32) for i in range(3)]
                ym = [sbb("Bym%d" % i, [128, D], F32) for i in range(2)]
                xo = [sbb("Bxo%d" % i, [128, D], F32) for i in range(2)]
                btiles = list(lat_tiles) + [32 + k for k in range(2 * NA)]
                NTB = len(btiles)

                def srcB(t):
                    return xown[t * 128:(t + 1) * 128, :] if t < 32 else xc[(t - 32) * 128:(t - 31) * 128, :]

                def issue_lb(n):
                    if n >= NTB:
                        return
                    i = n % 3
                    T.op('sp', lambda e: e.dma_start(out=xt[i][:], in_=srcB(btiles[n])), writes=[('Bxt', i)], dsem='Bxt%d' % i)
                issue_lb(0); issue_lb(1)
                for n in range(NTB):
                    t = btiles[n]
                    issue_lb(n + 2)
                    i = n % 3; o = n % 2
                    which = 0 if t < 32 else 1
                    for half in range(2):
                        bank = ps[(n % 2) * 2 + half]; bk = ('ps', (n % 2) * 2 + half)
                        for c in range(8):
                            lhs = YT[:, c, t * 128:(t + 1) * 128] if t < 32 else YTc[:, c, (t - 32) * 128:(t - 31) * 128]
                            T.op('pe', lambda e: e.matmul(bank[:], lhsT=lhs, rhs=Wo[:, c, half * 512:(half + 1) * 512], start=(c == 0), stop=(c == 7)),
                                 reads=['Wo'], writes=[bk])
                        T.op('dve', lambda e: e.tensor_tensor(out=ym[o][:, half * 512:(half + 1) * 512], in0=bank[:], in1=mods[(which, 2)][:, half * 512:(half + 1) * 512], op=ALU.mult),
                             reads=[bk, ('mod', which, 2)], writes=[('Bym', o, half)])
                    T.op('pool', lambda e: e.tensor_tensor(out=xo[o][:], in0=ym[o][:], in1=xt[i][:], op=ALU.add),
                         reads=[('Bym', o, 0), ('Bym', o, 1), ('Bxt', i)], writes=[('Bxo', o)])
                    T.op('sp', lambda e: e.dma_start(out=X1d[t * 128:(t + 1) * 128, :], in_=xo[o][:]), reads=[('Bxo', o)], writes=[('X1d', t)], dsem='x1w')
                T.barrier()
        if debug_stop == 'B1':
            return nc
        ffn_dense_phase(nc, T, ps, idb, mods, X1d, yown, yc, wgb, wub, wdb, lat_tiles=lat_tiles, n_ctx_tiles=2 * NA)
        T.barrier()


def ffn_dense_phase(nc, T, ps, idb, mods, X1d, yown, yc, wgb, wub, wdb, lat_tiles, n_ctx_tiles):
    with contextlib.ExitStack() as es:
        def sb(name, shape, dtp):
            return es.enter_context(nc.sbuf_tensor(_PFX[0] + name, shape, dtp))
        NC = NormCtx(nc, T, sb, idb, "F", nbuf=4)
        xt = [sb("Fxt%d" % i, [128, D], F32) for i in range(3)]
        hTs = [sb("FhT%d" % i, [128, 8, 512], BF16) for i in range(2)]
        h2T = sb("Fh2T", [128, NF, 512], BF16)
        wgs = [sb("Fwg%d" % i, [128, 8, 512], BF16) for i in range(2)]
        wus = [sb("Fwu%d" % i, [128, 8, 512], BF16) for i in range(2)]
        wds = [sb("Fwd%d" % i, [128, 1024], BF16) for i in range(4)]
        sg = [sb("Fsg%d" % i, [128, 512], F32) for i in range(2)]
        yt = [sb("Fyt%d" % i, [128, 512], F32) for i in range(2)]
        xr_ = [sb("Fxr%d" % i, [128, D], F32) for i in range(2)]
        xo = [sb("Fxo%d" % i, [128, D], F32) for i in range(2)]
        wgv = wgb.rearrange("(c p) f -> p c f", p=128)
        wuv = wub.rearrange("(c p) f -> p c f", p=128)
        n_lat_tiles = 32
        blocks = []
        lt = list(lat_tiles)
        for k in range(0, len(lt), 4):
            blocks.append((0, lt[k:k + 4]))
        if n_ctx_tiles:
            blocks.append((1, [32 + k for k in range(n_ctx_tiles)]))
        xcnt = [0]
        wcnt = [0]
        dcnt = [0]
        ecnt = [0]
        def norm_pre(bi):
            which, tiles = blocks[bi]
            slots = []
            for si, tt in enumerate(tiles):
                i = xcnt[0] % 3; xcnt[0] += 1
                T.op('sp', lambda e: e.dma_start(out=xt[i][:], in_=X1d[tt * 128:(tt + 1) * 128, :]), reads=[('X1d', tt)], writes=[('Fxt', i)], dsem='Fxt%d' % i)
                slots.append(NC.pre(xt[i][:], ('Fxt', i), mods[(which, 4)][:], ('mod', which, 4), mods[(which, 3)][:], ('mod', which, 3)))
            return slots

        def norm_post(bi, slots):
            hb_ = bi % 2
            for si, sl in enumerate(slots):
                NC.post(sl, ps[si % 2], ('ps', si % 2), hTs[hb_][:, :, si * 128:(si + 1) * 128], ('FhT', hb_, si), evac_eng='act')
        pend = norm_pre(0)
        norm_post(0, pend)
        for bi, (which, tiles) in enumerate(blocks):
            nt = len(tiles)
            ntok = nt * 128
            hT = hTs[bi % 2]
            hkeys = [('FhT', bi % 2, si) for si in range(nt)]
            for fg in range(7):
                wi = wcnt[0] % 2; wcnt[0] += 1
                T.op('sp', lambda e: e.dma_start(out=wgs[wi][:], in_=wgv[:, :, fg * 512:(fg + 1) * 512]), reads=['wgb'], writes=[('Fwg', wi)], dsem='Fwg%d' % wi)
                T.op('sp', lambda e: e.dma_start(out=wus[wi][:], in_=wuv[:, :, fg * 512:(fg + 1) * 512]), reads=['wub'], writes=[('Fwu', wi)], dsem='Fwu%d' % wi)
                for fi in range(4):
                    f = fg * 4 + fi
                    pb = (f % 2) * 2
                    for kc in range(8):
                        T.op('pe', lambda e: e.matmul(ps[pb][:, :ntok], lhsT=wgs[wi][:, kc, fi * 128:(fi + 1) * 128], rhs=hT[:, kc, :ntok], start=(kc == 0), stop=(kc == 7)),
                             reads=[('Fwg', wi)] + hkeys, writes=[('ps', pb)])
                    for kc in range(8):
                        T.op('pe', lambda e: e.matmul(ps[pb + 1][:, :ntok], lhsT=wus[wi][:, kc, fi * 128:(fi + 1) * 128], rhs=hT[:, kc, :ntok], start=(kc == 0), stop=(kc == 7)),
                             reads=[('Fwu', wi)] + hkeys, writes=[('ps', pb + 1)])
                    si_ = f % 2
                    T.op('act', lambda e: e.activation(out=sg[si_][:, :ntok], in_=ps[pb][:, :ntok], func=AF.Silu), reads=[('ps', pb)], writes=[('Fsg', si_)])
                    T.op('dve', lambda e: e.tensor_tensor(out=h2T[:, f, :ntok], in0=ps[pb + 1][:, :ntok], in1=sg[si_][:, :ntok], op=ALU.mult),
                         reads=[('ps', pb + 1), ('Fsg', si_)], writes=[('Fh2T', f)])
            nxt = norm_pre(bi + 1) if bi + 1 < len(blocks) else None
            for f in range(NF):
                di = dcnt[0] % 4; dcnt[0] += 1
                T.op('sp', lambda e: e.dma_start(out=wds[di][:], in_=wdb[f * 128:(f + 1) * 128, :]), reads=['wdb'], writes=[('Fwd', di)], dsem='Fwd%d' % di)
                for si in range(nt):
                    for half in range(2):
                        b = si * 2 + half
                        T.op('pe', lambda e: e.matmul(ps[b][:], lhsT=h2T[:, f, si * 128:(si + 1) * 128], rhs=wds[di][:, half * 512:(half + 1) * 512], start=(f == 0), stop=(f == NF - 1)),
                             reads=[('Fh2T', f), ('Fwd', di)], writes=[('ps', b)])
            for si, tt in enumerate(tiles):
                o = ecnt[0] % 2; ecnt[0] += 1
                T.op('sp', lambda e: e.dma_start(out=xr_[o][:], in_=X1d[tt * 128:(tt + 1) * 128, :]), reads=[('X1d', tt)], writes=[('Fxr', o)], dsem='Fxr%d' % o)
                for half in range(2):
                    b = si * 2 + half
                    T.op('dve', lambda e: e.tensor_tensor(out=yt[half][:], in0=ps[b][:], in1=mods[(which, 5)][:, half * 512:(half + 1) * 512], op=ALU.mult),
                         reads=[('ps', b), ('mod', which, 5)], writes=[('Fyt', half)])
                    T.op('pool', lambda e: e.tensor_tensor(out=xo[o][:, half * 512:(half + 1) * 512], in0=yt[half][:], in1=xr_[o][:, half * 512:(half + 1) * 512], op=ALU.add),
                         reads=[('Fyt', half), ('Fxr', o)], writes=[('Fxo', o, half)])
                dst = yown[tt * 128:(tt + 1) * 128, :] if tt < n_lat_tiles else yc[(tt - n_lat_tiles) * 128:(tt - n_lat_tiles + 1) * 128, :]
                T.op('sp', lambda e: e.dma_start(out=dst, in_=xo[o][:]), reads=[('Fxo', o, 0), ('Fxo', o, 1)], writes=[('yout', tt)], dsem='yout')
            if nxt is not None:
                norm_post(bi + 1, nxt)


def fnet_tables(s, shift=0):
    n = np.arange(256)
    ang = 2 * np.pi * np.outer(n, n) / 256.0
    cs256 = np.concatenate([np.cos(ang), np.sin(ang)], axis=1).astype(np.float32)
    m = np.arange(128)[:, None, None]; j = np.arange(64)[None, :, None]; kb = np.arange(128)[None, None, :]
    ph = (((j + 64 * m + shift) % 8192) * kb) % 8192
    a = 2 * np.pi * ph / 8192.0
    twc = np.cos(a).astype(np.float32).reshape(128, 64 * 128)
    tws = np.sin(a).astype(np.float32).reshape(128, 64 * 128)
    jj = np.arange(64)[:, None]; ka = (32 * s + np.arange(32))[None, :]
    a2 = 2 * np.pi * ((jj * ka) % 64) / 64.0
    w64 = np.concatenate([np.cos(a2), -np.sin(a2)], axis=0).astype(np.float32)
    return cs256, twc, tws, w64


NW = 34
NTOK = 36 * 128
CAP = 2048


def emit_attn(nc, T, ps, common, W, A, SC, s, do_ctx, final, cap=CAP, mod_load=False):
    debug_stop = None
    last = final
    idf, idb, ones = common
    xfull = A['xin']; xc = A['cin']
    yown = A['yout'][s * 4096:(s + 1) * 4096, :]; yc = A['cout']
    cvec = W['cvec']; ada_w = W['ada_w']; ada_b = W['ada_b']; nmg = W['nmg']; nfg = W['nfg']
    wq = W['wq']; wqr = W['wqr']; wk2 = W['wk2']; wk2r = W['wk2r']; wv = W['wv']; wo = W['wo']
    cost = W['cost_%d' % s]; sint = W['sint_%d' % s]; sinks = W['sinks']; masks = W['masks_%d' % s]; wr = W['wr']
    mwg = W['mwg']; mwu = W['mwu']; mwd = W['mwd']; fng = W['fng']; ut_d = W['ut']; ebase_d = W['ebase']
    hTd = SC['hTd']; qTd = SC['qTd']; X1d = SC['X1d']; hbuf = SC['hbuf']; ybuf = SC['ybuf']
    n_ctx_q = 2 if do_ctx else 0
    n_moe_tiles = 32 + n_ctx_q

    def win_tile(tt):
        r0 = (s * 4096 - 128 + tt * 128) % S
        return xfull[r0:r0 + 128, :]

    with contextlib.ExitStack() as es:
        def sbg(name, shape, dtp):
            return es.enter_context(nc.sbuf_tensor(_PFX[0] + name, shape, dtp))
        mods = {}
        for which in range(2):
            for idx in (3, 4, 5):
                mods[(which, idx)] = sbg("mod%d_%d" % (which, idx), [128, D], F32)
        with contextlib.ExitStack() as es_mix:
            def sbm(name, shape, dtp):
                return es_mix.enter_context(nc.sbuf_tensor(_PFX[0] + name, shape, dtp))
            for which in range(2):
                for idx in (0, 1, 2):
                    mods[(which, idx)] = sbm("mod%d_%d" % (which, idx), [128, D], F32)
            compute_mod(nc, T, None, ps, cvec, ada_w, ada_b, ones, mods,
                        [(nmg, [(0, 1), (1, 1)]), (nfg, [(0, 4), (1, 4)])], cache=SC['modc'], load=mod_load)
            kT2 = sbm("kT2", [128, 4, NTOK], BF16)
            V = sbm("V", [128, 36, 256], BF16)
            with contextlib.ExitStack() as esK:
                def sbk(name, shape, dtp):
                    return esK.enter_context(nc.sbuf_tensor(_PFX[0] + name, shape, dtp))
                NC = NormCtx(nc, T, sbk, idb, "K")
                Wk2 = sbk("Wk2", [128, 8, 512], BF16); Wk2r = sbk("Wk2r", [128, 8, 512], BF16); Wv = sbk("Wv", [128, 8, 256], BF16)
                T.op('pool', lambda e: e.dma_start(out=Wk2[:], in_=wk2.rearrange("(c p) f -> p c f", p=128)), writes=['Wk2'], dsem='c1')
                T.op('pool', lambda e: e.dma_start(out=Wk2r[:], in_=wk2r.rearrange("(c p) f -> p c f", p=128)), writes=['Wk2r'], dsem='c1')
                T.op('pool', lambda e: e.dma_start(out=Wv[:], in_=wv.rearrange("(c p) f -> p c f", p=128)), writes=['Wv'], dsem='c1')
                xt = [sbk("Kxt%d" % i, [128, D], F32) for i in range(3)]
                hT = [sbk("KhT%d" % i, [128, 8, 512], BF16) for i in range(2)]
                ct = [sbk("Kct%d" % i, [128, 512], F32) for i in range(2)]
                st = [sbk("Kst%d" % i, [128, 512], F32) for i in range(2)]
                t1 = [sbk("Kt1_%d" % i, [128, 512], F32) for i in range(2)]
                t2 = [sbk("Kt2_%d" % i, [128, 512], F32) for i in range(2)]
                xcnt = 0
                for g in range(9):
                    hi = g % 2
                    T.op('sp', lambda e: e.dma_start(out=ct[hi][:], in_=cost[:, g * 512:(g + 1) * 512]), writes=[('Kct', hi)], dsem='Kct%d' % hi)
                    T.op('sp', lambda e: e.dma_start(out=st[hi][:], in_=sint[:, g * 512:(g + 1) * 512]), writes=[('Kst', hi)], dsem='Kst%d' % hi)
                    for si in range(4):
                        tt = g * 4 + si
                        which = 0 if tt < NW else 1
                        src = win_tile(tt) if tt < NW else xc[(tt - NW) * 128:(tt - NW + 1) * 128, :]
                        i = xcnt % 3; xcnt += 1
                        T.op('sp', lambda e: e.dma_start(out=xt[i][:], in_=src), writes=[('Kxt', i)], dsem='Kxt%d' % i)
                        NC.run(xt[i][:], ('Kxt', i), mods[(which, 1)][:], ('mod', which, 1), mods[(which, 0)][:], ('mod', which, 0),
                               ps[6 + si % 2], ('ps', 6 + si % 2), hT[hi][:, :, si * 128:(si + 1) * 128], ('KhT', hi, si), evac_eng='act')
                        bank = ps[4 + si % 2]; bk = ('ps', 4 + si % 2)
                        for kc in range(8):
                            T.op('pe', lambda e: e.matmul(bank[:, 0:256], lhsT=hT[hi][:, kc, si * 128:(si + 1) * 128], rhs=Wv[:, kc, :], start=(kc == 0), stop=(kc == 7)),
                                 reads=[('KhT', hi, si), 'Wv'], writes=[bk])
                        T.op('act', lambda e: e.activation(out=V[:, tt, :], in_=bank[:, 0:256], func=AF.Copy), reads=[bk], writes=[('V', tt)])
                    hkeys = [('KhT', hi, si) for si in range(4)]
                    T.op('sp', lambda e: e.dma_start(out=hTd[:, :, g * 512:(g + 1) * 512], in_=hT[hi][:]), reads=hkeys, writes=[('hTd', g)], dsem='htw')
                    for kv in range(4):
                        b0 = ps[(kv % 2) * 2]; k0 = ('ps', (kv % 2) * 2)
                        b1 = ps[(kv % 2) * 2 + 1]; k1 = ('ps', (kv % 2) * 2 + 1)
                        for kc in range(8):
                            T.op('pe', lambda e: e.matmul(b0[:], lhsT=Wk2[:, kc, kv * 128:(kv + 1) * 128], rhs=hT[hi][:, kc, :], start=(kc == 0), stop=(kc == 7)),
                                 reads=hkeys + ['Wk2'], writes=[k0])
                        for kc in range(8):
                            T.op('pe', lambda e: e.matmul(b1[:], lhsT=Wk2r[:, kc, kv * 128:(kv + 1) * 128], rhs=hT[hi][:, kc, :], start=(kc == 0), stop=(kc == 7)),
                                 reads=hkeys + ['Wk2r'], writes=[k1])
                        ti = kv % 2
                        T.op('dve', lambda e: e.tensor_tensor(out=t1[ti][:], in0=b0[:], in1=ct[hi][:], op=ALU.mult), reads=[k0, ('Kct', hi)], writes=[('Kt1', ti)])
                        T.op('dve', lambda e: e.tensor_tensor(out=t2[ti][:], in0=b1[:], in1=st[hi][:], op=ALU.mult), reads=[k1, ('Kst', hi)], writes=[('Kt2', ti)])
                        T.op('pool', lambda e: e.tensor_tensor(out=kT2[:, kv, g * 512:(g + 1) * 512], in0=t1[ti][:], in1=t2[ti][:], op=ALU.add),
                             reads=[('Kt1', ti), ('Kt2', ti)], writes=[('kT2', kv, g)])
                T.barrier()
            if debug_stop == 'K':
                return nc
            with contextlib.ExitStack() as esQ:
                def sbq(name, shape, dtp):
                    return esQ.enter_context(nc.sbuf_tensor(_PFX[0] + name, shape, dtp))
                Wq = sbq("Wq", [128, 8, 1024], BF16); Wqr = sbq("Wqr", [128, 8, 1024], BF16)
                T.op('pool', lambda e: e.dma_start(out=Wq[:], in_=wq.rearrange("(c p) f -> p c f", p=128)), writes=['Wq'], dsem='c1')
                T.op('pool', lambda e: e.dma_start(out=Wqr[:], in_=wqr.rearrange("(c p) f -> p c f", p=128)), writes=['Wqr'], dsem='c1')
                hT = [sbq("QhT%d" % i, [128, 8, 512], BF16) for i in range(2)]
                qo = [sbq("Qqo%d" % i, [128, 8, 512], BF16) for i in range(2)]
                ct = [sbq("Qct%d" % i, [128, 512], F32) for i in range(2)]
                st = [sbq("Qst%d" % i, [128, 512], F32) for i in range(2)]
                t1 = [sbq("Qt1_%d" % i, [128, 512], F32) for i in range(2)]
                t2 = [sbq("Qt2_%d" % i, [128, 512], F32) for i in range(2)]
                groups = [(128 + 512 * g, 512, 512 * g) for g in range(8)]
                if n_ctx_q:
                    groups.append((NW * 128, 256, 4096))
                for gi, (tok0, n, qoff) in enumerate(groups):
                    hi = gi % 2
                    T.op('sp', lambda e: e.dma_start(out=hT[hi][:, :, :n], in_=hTd[:, :, tok0:tok0 + n]), reads=[('hTd', g) for g in range(9)], writes=[('QhT', hi)], dsem='QhT%d' % hi)
                    T.op('sp', lambda e: e.dma_start(out=ct[hi][:, :n], in_=cost[:, tok0:tok0 + n]), writes=[('Qct', hi)], dsem='Qct%d' % hi)
                    T.op('sp', lambda e: e.dma_start(out=st[hi][:, :n], in_=sint[:, tok0:tok0 + n]), writes=[('Qst', hi)], dsem='Qst%d' % hi)
                    for c in range(8):
                        b0 = ps[(c % 4) * 2]; k0 = ('ps', (c % 4) * 2)
                        b1 = ps[(c % 4) * 2 + 1]; k1 = ('ps', (c % 4) * 2 + 1)
                        for kc in range(8):
                            T.op('pe', lambda e: e.matmul(b0[:, :n], lhsT=Wq[:, kc, c * 128:(c + 1) * 128], rhs=hT[hi][:, kc, :n], start=(kc == 0), stop=(kc == 7)),
                                 reads=[('QhT', hi), 'Wq'], writes=[k0])
                        for kc in range(8):
                            T.op('pe', lambda e: e.matmul(b1[:, :n], lhsT=Wqr[:, kc, c * 128:(c + 1) * 128], rhs=hT[hi][:, kc, :n], start=(kc == 0), stop=(kc == 7)),
                                 reads=[('QhT', hi), 'Wqr'], writes=[k1])
                        ti = c % 2
                        T.op('dve', lambda e: e.tensor_tensor(out=t1[ti][:, :n], in0=b0[:, :n], in1=ct[hi][:, :n], op=ALU.mult), reads=[k0, ('Qct', hi)], writes=[('Qt1', ti)])
                        T.op('dve', lambda e: e.tensor_tensor(out=t2[ti][:, :n], in0=b1[:, :n], in1=st[hi][:, :n], op=ALU.mult), reads=[k1, ('Qst', hi)], writes=[('Qt2', ti)])
                        T.op('pool', lambda e: e.tensor_tensor(out=qo[hi][:, c, :n], in0=t1[ti][:, :n], in1=t2[ti][:, :n], op=ALU.add),
                             reads=[('Qt1', ti), ('Qt2', ti)], writes=[('Qqo', hi, c)])
                    T.op('sp', lambda e: e.dma_start(out=qTd[:, :, qoff:qoff + n], in_=qo[hi][:, :, :n]), reads=[('Qqo', hi, c) for c in range(8)], writes=[('qTd', gi)], dsem='qtw')
                T.barrier()
            if debug_stop == 'Q':
                return nc
            with contextlib.ExitStack() as esA:
                def sba(name, shape, dtp):
                    return esA.enter_context(nc.sbuf_tensor(_PFX[0] + name, shape, dtp))
                Wo = sba("Wo", [64, 16, 1024], BF16)
                T.op('pool', lambda e: e.dma_start(out=Wo[:], in_=wo.rearrange("(h d) f -> d h f", d=64)), writes=['Wo'], dsem='c1')
                mk = sba("mk", [128, 512], BF16)
                T.op('pool', lambda e: e.dma_start(out=mk[:], in_=masks), writes=['mk'], dsem='c1')
                sk = sba("sk", [64, 16], F32); esk = sba("esk", [64, 16], F32)
                T.op('sp', lambda e: e.dma_start(out=sk[:], in_=sinks.partition_broadcast(64).squeeze(1)), writes=['sk'], dsem='c0')
                T.op('act', lambda e: e.activation(out=esk[:], in_=sk[:], func=AF.Exp), reads=['sk'], writes=['esk'])
                ES = sba("ES", [64, 4, 512], F32)
                for kv in range(4):
                    for slot in range(4):
                        half, cc = slot // 2, slot % 2
                        hd = 4 * kv + 2 * cc + half
                        T.op('dve', lambda e: e.tensor_scalar(out=ES[:, kv, slot * 128:(slot + 1) * 128], in0=ones[0:64, :], scalar1=esk[:, hd:hd + 1], scalar2=None, op0=ALU.mult),
                             reads=['ones', 'esk'], writes=[('ES', kv, slot)])
                ones_b = sba("ones_b", [128, 64], BF16)
                T.op('pool', lambda e: e.memset(ones_b[:], 1.0), writes=['ones_b'])
                qt = [sba("Aqt%d" % i, [128, 8, 512], BF16) for i in range(2)]
                PT = [sba("APT%d" % i, [128, 512], BF16) for i in range(6)]
                oT = [sba("AoT%d" % i, [64, 4, 512], BF16) for i in range(2)]
                dn = [sba("Adn%d" % i, [64, 512], F32) for i in range(2)]
                xt = [sba("Axt%d" % i, [128, D], F32) for i in range(2)]
                ym = [sba("Aym%d" % i, [128, D], F32) for i in range(2)]
                xo = [sba("Axo%d" % i, [128, D], F32) for i in range(2)]
                ptc = 0
                nqb = 32 + n_ctx_q
                qkeys = [('qTd', g) for g in range(9)]
                for qb in range(nqb):
                    qg = qb // 4
                    qi = qg % 2
                    if qb % 4 == 0:
                        n = 512 if qb < 32 else 256
                        T.op('sp', lambda e: e.dma_start(out=qt[qi][:, :, :n], in_=qTd[:, :, qb * 128: qb * 128 + n]), reads=qkeys, writes=[('Aqt', qi)], dsem='Aqt%d' % qi)
                    qo_ = (qb % 4) * 128
                    is_ctx = qb >= 32
                    w = qb + 1
                    if is_ctx:
                        chunks = [(34, None), (35, None)]
                    else:
                        mP = 1 if qb == 0 else 0
                        mN = 3 if qb == 31 else 2
                        chunks = [(w - 1, mP), (w, None), (w + 1, mN), (34, None), (35, None)]
                    oi = qb % 2
                    for kv in range(4):
                        pts = []
                        for (kt, mi) in chunks:
                            sb_i = (ptc % 2) * 2
                            pi = ptc % 6; ptc += 1
                            for half in range(2):
                                bank = ps[sb_i + half]; bk = ('ps', sb_i + half)
                                T.op('pe', lambda e: e.matmul(bank[:, 0:256].rearrange("p (c q) -> p c q", c=2),
                                                              lhsT=kT2[half * 64:(half + 1) * 64, kv, kt * 128:(kt + 1) * 128],
                                                              rhs=qt[qi][half * 64:(half + 1) * 64, 2 * kv:2 * kv + 2, qo_:qo_ + 128], start=True, stop=True),
                                     reads=[('Aqt', qi)], writes=[bk])
                                T.op('act', lambda e: e.activation(out=PT[pi][:, half * 256:(half + 1) * 256], in_=bank[:, 0:256], func=AF.Exp, scale=0.125), reads=[bk], writes=[('APT', pi, half)])
                            if mi is not None:
                                T.op('pool', lambda e: e.tensor_tensor(out=PT[pi][:].rearrange("p (s q) -> p s q", s=4), in0=PT[pi][:].rearrange("p (s q) -> p s q", s=4),
                                                                      in1=mk[:, mi * 128:(mi + 1) * 128].unsqueeze(1).to_broadcast([128, 4, 128]), op=ALU.mult),
                                     reads=[('APT', pi, 0), ('APT', pi, 1), 'mk'], writes=[('APT', pi, 0), ('APT', pi, 1)])
                            pts.append((pi, kt))
                        ob = ps[4 + kv % 2]; obk = ('ps', 4 + kv % 2)
                        db = ps[6 + kv % 2]; dbk = ('ps', 6 + kv % 2)
                        for ci, (pi, kt) in enumerate(pts):
                            T.op('pe', lambda e: e.matmul(ob[0:64, :], lhsT=V[:, kt, kv * 64:(kv + 1) * 64], rhs=PT[pi][:], start=(ci == 0), stop=(ci == len(pts) - 1)),
                                 reads=[('APT', pi, 0), ('APT', pi, 1)], writes=[obk])
                        for ci, (pi, kt) in enumerate(pts):
                            T.op('pe', lambda e: e.matmul(db[0:64, :], lhsT=ones_b[:, :], rhs=PT[pi][:], start=(ci == 0), stop=(ci == len(pts) - 1)),
                                 reads=[('APT', pi, 0), ('APT', pi, 1), 'ones_b'], writes=[dbk])
                        di = kv % 2
                        T.op('dve', lambda e: e.tensor_tensor(out=dn[di][:], in0=db[0:64, :], in1=ES[:, kv, :], op=ALU.add), reads=[dbk, ('ES', kv, 0), ('ES', kv, 1), ('ES', kv, 2), ('ES', kv, 3)], writes=[('Adn', di)])
                        T.op('dve', lambda e: e.reciprocal(out=dn[di][:], in_=dn[di][:]), reads=[('Adn', di)], writes=[('Adn', di)])
                        T.op('dve', lambda e: e.tensor_tensor(out=oT[oi][:, kv, :], in0=ob[0:64, :], in1=dn[di][:], op=ALU.mult), reads=[obk, ('Adn', di)], writes=[('AoT', oi, kv)])
                    xi = qb % 2
                    src = win_tile(w) if not is_ctx else xc[(qb - 32) * 128:(qb - 31) * 128, :]
                    which = 1 if is_ctx else 0
                    T.op('sp', lambda e: e.dma_start(out=xt[xi][:], in_=src), writes=[('Axt', xi)], dsem='Axt%d' % xi)
                    for hf in range(2):
                        yb = ps[4 + hf] if False else ps[(qb % 2) * 2 + hf]
                        ybk = ('ps', (qb % 2) * 2 + hf)
                        n_acc = 0
                        for kv in range(4):
                            for slot in range(4):
                                half, cc = slot // 2, slot % 2
                                hd = 4 * kv + 2 * cc + half
                                T.op('pe', lambda e: e.matmul(yb[:], lhsT=oT[oi][:, kv, slot * 128:(slot + 1) * 128], rhs=Wo[:, hd, hf * 512:(hf + 1) * 512], start=(n_acc == 0), stop=(n_acc == 15)),
                                     reads=[('AoT', oi, kv), 'Wo'], writes=[ybk])
                                n_acc += 1
                        T.op('dve', lambda e: e.tensor_tensor(out=ym[xi][:, hf * 512:(hf + 1) * 512], in0=yb[:], in1=mods[(which, 2)][:, hf * 512:(hf + 1) * 512], op=ALU.mult),
                             reads=[ybk, ('mod', which, 2)], writes=[('Aym', xi, hf)])
                    T.op('pool', lambda e: e.tensor_tensor(out=xo[xi][:], in0=ym[xi][:], in1=xt[xi][:], op=ALU.add), reads=[('Aym', xi, 0), ('Aym', xi, 1), ('Axt', xi)], writes=[('Axo', xi)])
                    T.op('sp', lambda e: e.dma_start(out=X1d[qb * 128:(qb + 1) * 128, :], in_=xo[xi][:]), reads=[('Axo', xi)], writes=[('X1d', qb)], dsem='x1w')
                T.barrier()
        if debug_stop == 'ATT':
            with contextlib.ExitStack() as esD:
                tmp = esD.enter_context(nc.sbuf_tensor(_PFX[0] + "dbg", [128, D], F32))
                for t in range(34):
                    T.op('sp', lambda e: e.dma_start(out=tmp[:], in_=X1d[t * 128:(t + 1) * 128, :]), writes=['dbg'], dsem='dbg')
                    dst = yown[t * 128:(t + 1) * 128, :] if t < 32 else yc[(t - 32) * 128:(t - 31) * 128, :]
                    T.op('sp', lambda e: e.dma_start(out=dst, in_=tmp[:]), reads=['dbg'], writes=[('o', t)], dsem='dbg2')
                T.barrier()
            return nc
        moe_phase(nc, T, ps, idb, idf, ones, mods, X1d, yown, yc, wr, mwg, mwu, mwd, hbuf, ybuf, fng, n_moe_tiles, last, cap, ut_d, ebase_d)
        T.barrier()


def moe_phase(nc, T, ps, idb, idf, ones, mods, X1d, yown, yc, wr, mwg, mwu, mwd, hbuf, ybuf, fng, ntiles, last, cap, ut_d, ebase_d):
    with contextlib.ExitStack() as es0:
        def sb0(name, shape, dtp):
            return es0.enter_context(nc.sbuf_tensor(_PFX[0] + name, shape, dtp))
        bc_reg = nc.gpsimd.to_reg(8 * cap - 1)
        dest = sb0("dest", [128, ntiles * 2], I32)
        gate = sb0("gate", [128, ntiles * 2], F32)
        with contextlib.ExitStack() as es:
            def sb(name, shape, dtp):
                return es.enter_context(nc.sbuf_tensor(_PFX[0] + name, shape, dtp))
            NC = NormCtx(nc, T, sb, idb, "M", nbuf=4)
            Wr = sb("Wr", [128, 8, 8], F32)
            T.op('sp', lambda e: e.dma_start(out=Wr[:], in_=wr.rearrange("(c p) f -> p c f", p=128)), writes=['Wr'], dsem='c0')
            UT = sb("UT", [128, 128], F32)
            T.op('sp', lambda e: e.dma_start(out=UT[:], in_=ut_d), writes=['UT'], dsem='c0')
            ebase = sb("ebase_sb", [128, 8], F32)
            T.op('sp', lambda e: e.dma_start(out=ebase[:], in_=ebase_d), writes=[('ebase', ex) for ex in range(8)], dsem='c0')
            cnt = sb("cnt", [128, 8], F32)
            T.op('pool', lambda e: e.memset(cnt[:], 0.0), writes=['cnt'])
            xt = [sb("Mxt%d" % i, [128, D], F32) for i in range(4)]
            hTf = [sb("MhTf%d" % i, [128, 8, 128], F32) for i in range(4)]
            lg = [sb("Mlg%d" % i, [128, 8], F32) for i in range(4)]
            m8 = [sb("Mm8%d" % i, [128, 8], F32) for i in range(4)]
            sel = [sb("Msel%d" % i, [128, 8], F32) for i in range(4)]
            mk1 = [sb("Mmk1%d" % i, [128, 8], F32) for i in range(4)]
            mk2 = [sb("Mmk2%d" % i, [128, 8], F32) for i in range(4)]
            rk = [sb("Mrk%d" % i, [128, 8], F32) for i in range(4)]
            tmp8 = [sb("Mt8%d" % i, [128, 8], F32) for i in range(4)]
            df = [sb("Mdf%d" % i, [128, 2], F32) for i in range(4)]
            ex_ = [sb("Mex%d" % i, [128, 2], F32) for i in range(4)]
            ekeys = [('ebase', ex) for ex in range(8)]
            for t in range(ntiles):
                which = 0 if t < 32 else 1
                i = t % 4; o = t % 4
                T.op('sp', lambda e: e.dma_start(out=xt[i][:], in_=X1d[t * 128:(t + 1) * 128, :]), reads=[('X1d', t)], writes=[('Mxt', i)], dsem='Mxt%d' % i)
                r = NC.r.next()
                ss, h1, hb = NC.ss[r], NC.h1[r], NC.hb[r]
                T.op('act', lambda e: e.activation(out=NC.junk[:], in_=xt[i][:], func=AF.Square, accum_out=ss[:, 0:1]), reads=[('Mxt', i)], writes=[('Mss', r)])
                T.op('dve', lambda e: e.tensor_scalar(out=ss[:, 1:2], in0=ss[:, 0:1], scalar1=1.0 / D, scalar2=EPS, op0=ALU.mult, op1=ALU.add), reads=[('Mss', r)], writes=[('Mss1', r)])
                T.op('pool', lambda e: e.tensor_tensor(out=ss[:, 1:2], in0=ss[:, 1:2], in1=NC.nhalf[:], op=ALU.pow), reads=[('Mss1', r), 'Mnhalf'], writes=[('Mss1', r)])
                T.op('dve', lambda e: e.scalar_tensor_tensor(out=h1[:], in0=xt[i][:], scalar=ss[:, 1:2], in1=mods[(which, 4)][:], op0=ALU.mult, op1=ALU.mult),
                     reads=[('Mxt', i), ('Mss1', r), ('mod', which, 4)], writes=[('Mh1', r)])
                T.op('pool', lambda e: e.tensor_tensor(out=h1[:], in0=h1[:], in1=mods[(which, 3)][:], op=ALU.add), reads=[('Mh1', r), ('mod', which, 3)], writes=[('Mh1', r)])
                T.op('act', lambda e: e.activation(out=hb[:], in_=h1[:], func=AF.Copy), reads=[('Mh1', r)], writes=[('Mhb', r)])
                for half in range(2):
                    bank = ps[(t % 2) * 2 + half]; bk = ('ps', (t % 2) * 2 + half)
                    for c in range(4):
                        cc = half * 4 + c
                        T.op('pe', lambda e: e.transpose(out=bank[:, c * 128:(c + 1) * 128], in_=h1[:, cc * 128:(cc + 1) * 128], identity=idf[:]), reads=[('Mh1', r), 'idf'], writes=[bk])
                    T.op('dve' if half == 0 else 'act',
                         (lambda e: e.tensor_copy(out=hTf[o][:, 0:4, :], in_=bank[:].rearrange("p (c t) -> p c t", c=4))) if half == 0 else
                         (lambda e: e.activation(out=hTf[o][:, 4:8, :], in_=bank[:].rearrange("p (c t) -> p c t", c=4), func=AF.Copy)),
                         reads=[bk], writes=[('MhTf', o, half)])
                lb = ps[4 + t % 2]; lbk = ('ps', 4 + t % 2)
                for kc in range(8):
                    T.op('pe', lambda e: e.matmul(lb[:, 0:8], lhsT=hTf[o][:, kc, :], rhs=Wr[:, kc, :], start=(kc == 0), stop=(kc == 7)), reads=[('MhTf', o, kc // 4), 'Wr'], writes=[lbk])
                T.op('dve', lambda e: e.tensor_copy(out=lg[o][:], in_=lb[:, 0:8]), reads=[lbk], writes=[('Mlg', o)])
                T.op('dve', lambda e: e.max(out=m8[o][:], in_=lg[o][:]), reads=[('Mlg', o)], writes=[('Mm8', o)])
                T.op('dve', lambda e: e.tensor_scalar(out=mk1[o][:], in0=lg[o][:], scalar1=m8[o][:, 0:1], scalar2=None, op0=ALU.is_equal), reads=[('Mlg', o), ('Mm8', o)], writes=[('Mmk1', o)])
                T.op('dve', lambda e: e.tensor_scalar(out=mk2[o][:], in0=lg[o][:], scalar1=m8[o][:, 1:2], scalar2=None, op0=ALU.is_equal), reads=[('Mlg', o), ('Mm8', o)], writes=[('Mmk2', o)])
                T.op('dve', lambda e: e.tensor_tensor(out=sel[o][:], in0=mk1[o][:], in1=mk2[o][:], op=ALU.add), reads=[('Mmk1', o), ('Mmk2', o)], writes=[('Msel', o)])
                T.op('dve', lambda e: e.tensor_tensor(out=df[o][:, 0:1], in0=m8[o][:, 1:2], in1=m8[o][:, 0:1], op=ALU.subtract), reads=[('Mm8', o)], writes=[('Mdf', o)])
                T.op('act', lambda e: e.activation(out=ex_[o][:, 0:1], in_=df[o][:, 0:1], func=AF.Exp), reads=[('Mdf', o)], writes=[('Mex', o)])
                T.op('dve', lambda e: e.tensor_scalar(out=ex_[o][:, 1:2], in0=ex_[o][:, 0:1], scalar1=1.0, scalar2=None, op0=ALU.add), reads=[('Mex', o)], writes=[('Mex1', o)])
                T.op('dve', lambda e: e.reciprocal(out=gate[:, 2 * t:2 * t + 1], in_=ex_[o][:, 1:2]), reads=[('Mex1', o)], writes=[('gate', t, 0)])
                T.op('dve', lambda e: e.tensor_scalar(out=gate[:, 2 * t + 1:2 * t + 2], in0=gate[:, 2 * t:2 * t + 1], scalar1=-1.0, scalar2=1.0, op0=ALU.mult, op1=ALU.add), reads=[('gate', t, 0)], writes=[('gate', t, 1)])
                rb = ps[6 + t % 2]; rbk = ('ps', 6 + t % 2)
                T.op('pe', lambda e: e.matmul(rb[:, 0:8], lhsT=UT[:], rhs=sel[o][:], start=True, stop=True), reads=['UT', ('Msel', o)], writes=[rbk])
                T.op('pe', lambda e: e.matmul(rb[:, 8:16], lhsT=ones[:], rhs=sel[o][:], start=True, stop=True), reads=['ones', ('Msel', o)], writes=[rbk])
                T.op('dve', lambda e: e.tensor_tensor(out=rk[o][:], in0=rb[:, 0:8], in1=cnt[:], op=ALU.add), reads=[rbk, 'cnt'], writes=[('Mrk', o)])
                T.op('dve', lambda e: e.tensor_tensor(out=cnt[:], in0=rb[:, 8:16], in1=cnt[:], op=ALU.add), reads=[rbk, 'cnt', ('Mrk', o)], writes=['cnt'])
                T.op('dve', lambda e: e.tensor_scalar(out=tmp8[o][:], in0=rk[o][:], scalar1=float(cap), scalar2=1.0e7, op0=ALU.is_ge, op1=ALU.mult), reads=[('Mrk', o)], writes=[('Mt8', o)])
                T.op('dve', lambda e: e.tensor_tensor(out=rk[o][:], in0=rk[o][:], in1=tmp8[o][:], op=ALU.add), reads=[('Mrk', o), ('Mt8', o)], writes=[('Mrk', o)])
                T.op('dve', lambda e: e.tensor_tensor(out=rk[o][:], in0=rk[o][:], in1=ebase[:], op=ALU.add), reads=[('Mrk', o)] + ekeys, writes=[('Mrk', o)])
                for k, mk_ in enumerate((mk1, mk2)):
                    T.op('dve', lambda e: e.tensor_tensor(out=tmp8[o][:], in0=rk[o][:], in1=mk_[o][:], op=ALU.mult), reads=[('Mrk', o), ('Mmk%d' % (k + 1), o)], writes=[('Mt8', o)])
                    T.op('dve', lambda e: e.tensor_reduce(out=df[o][:, 1:2], in_=tmp8[o][:], axis=AX.X, op=ALU.add), reads=[('Mt8', o)], writes=[('Mdf1', o)])
                    T.op('dve', lambda e: e.tensor_copy(out=dest[:, 2 * t + k:2 * t + k + 1], in_=df[o][:, 1:2]), reads=[('Mdf1', o)], writes=[('dest', t, k)])
                    T.op('pool', lambda e: e.indirect_dma_start(out=hbuf[:, :], out_offset=bass.IndirectOffsetOnAxis(ap=dest[:, 2 * t + k:2 * t + k + 1], axis=0), in_=hb[:, :], in_offset=None,
                                                                bounds_check=bc_reg, oob_is_err=False),
                         reads=[('dest', t, k), ('Mhb', r)], writes=[('hbuf', t, k)], dsem='hsc')
            T.barrier()
        nst = cap // 512
        with contextlib.ExitStack() as es:
            def sb(name, shape, dtp):
                return es.enter_context(nc.sbuf_tensor(_PFX[0] + name, shape, dtp))
            hTe = sb("EhT", [128, 8, cap], BF16)
            acc = sb("Eacc", [128, cap // 128, D], F32)
            hg = [sb("Ehg%d" % i, [128, D], BF16) for i in range(3)]
            wgs = [sb("Ewg%d" % i, [128, 8, 512], BF16) for i in range(2)]
            wus = [sb("Ewu%d" % i, [128, 8, 512], BF16) for i in range(2)]
            wds = [sb("Ewd%d" % i, [128, 4, D], BF16) for i in range(2)]
            h2 = [sb("Eh2_%d" % i, [128, 4, 512], BF16) for i in range(2)]
            sg = [sb("Esg%d" % i, [128, 512], F32) for i in range(2)]
            wcnt = 0; gcnt = 0; hcnt = 0; bcnt = 0
            for ex in range(8):
                for s_ in range(cap // 128):
                    gi = gcnt % 3; gcnt += 1
                    T.op('sp', lambda e: e.dma_start(out=hg[gi][:], in_=hbuf[ex * cap + s_ * 128: ex * cap + (s_ + 1) * 128, :]), reads=['hbuf_all'], writes=[('Ehg', gi)], dsem='Ehg%d' % gi)
                    bank = ps[6 + s_ % 2]; bk = ('ps', 6 + s_ % 2)
                    pT = bank[:].bitcast(BF16).rearrange("p (c t) -> p c t", c=8)
                    for c in range(8):
                        T.op('pe', lambda e: e.transpose(out=pT[:, c, :], in_=hg[gi][:, c * 128:(c + 1) * 128], identity=idb[:]), reads=[('Ehg', gi), 'idb'], writes=[bk])
                    if s_ % 2 == 0:
                        T.op('act', lambda e: e.activation(out=hTe[:, :, s_ * 128:(s_ + 1) * 128], in_=pT, func=AF.Copy), reads=[bk], writes=[('EhT', s_)])
                    else:
                        T.op('dve', lambda e: e.tensor_copy(out=hTe[:, :, s_ * 128:(s_ + 1) * 128], in_=pT), reads=[bk], writes=[('EhT', s_)])
                for fg in range(7):
                    wi = wcnt % 2; wcnt += 1
                    T.op('pool', lambda e: e.dma_start(out=wgs[wi][:], in_=mwg[ex].rearrange("(c p) f -> p c f", p=128)[:, :, fg * 512:(fg + 1) * 512]), writes=[('Ewg', wi)], dsem='Ewg%d' % wi)
                    T.op('pool', lambda e: e.dma_start(out=wus[wi][:], in_=mwu[ex].rearrange("(c p) f -> p c f", p=128)[:, :, fg * 512:(fg + 1) * 512]), writes=[('Ewu', wi)], dsem='Ewu%d' % wi)
                    T.op('pool', lambda e: e.dma_start(out=wds[wi][:], in_=mwd[ex][fg * 512:(fg + 1) * 512, :].rearrange("(c p) f -> p c f", p=128)), writes=[('Ewd', wi)], dsem='Ewd%d' % wi)
                    for st_ in range(nst):
                        hi = hcnt % 2; hcnt += 1
                        hk = [('EhT', st_ * 4 + q) for q in range(4)]
                        for fi in range(4):
                            pb = (bcnt % 2) * 2; bcnt += 1
                            for kc in range(8):
                                T.op('pe', lambda e: e.matmul(ps[pb][:], lhsT=wgs[wi][:, kc, fi * 128:(fi + 1) * 128], rhs=hTe[:, kc, st_ * 512:(st_ + 1) * 512], start=(kc == 0), stop=(kc == 7)),
                                     reads=[('Ewg', wi)] + hk, writes=[('ps', pb)])
                            for kc in range(8):
                                T.op('pe', lambda e: e.matmul(ps[pb + 1][:], lhsT=wus[wi][:, kc, fi * 128:(fi + 1) * 128], rhs=hTe[:, kc, st_ * 512:(st_ + 1) * 512], start=(kc == 0), stop=(kc == 7)),
                                     reads=[('Ewu', wi)] + hk, writes=[('ps', pb + 1)])
                            si_ = fi % 2
                            T.op('act', lambda e: e.activation(out=sg[si_][:], in_=ps[pb][:], func=AF.Silu), reads=[('ps', pb)], writes=[('Esg', si_)])
                            T.op('dve', lambda e: e.tensor_tensor(out=h2[hi][:, fi, :], in0=ps[pb + 1][:], in1=sg[si_][:], op=ALU.mult), reads=[('ps', pb + 1), ('Esg', si_)], writes=[('Eh2', hi, fi)])
                        for sub in range(4):
                            for half in range(2):
                                ab = 4 + ((sub * 2 + half) % 4); abk = ('ps', ab)
                                for fi in range(4):
                                    T.op('pe', lambda e: e.matmul(ps[ab][:], lhsT=h2[hi][:, fi, sub * 128:(sub + 1) * 128], rhs=wds[wi][:, fi, half * 512:(half + 1) * 512], start=(fi == 0), stop=(fi == 3)),
                                         reads=[('Eh2', hi, fi), ('Ewd', wi)], writes=[abk])
                                sl = st_ * 4 + sub
                                dsta = acc[:, sl, half * 512:(half + 1) * 512]
                                if fg == 0:
                                    T.op('act', lambda e: e.activation(out=dsta, in_=ps[ab][:], func=AF.Copy), reads=[abk], writes=[('Eacc', sl, half)])
                                else:
                                    T.op('dve', lambda e: e.tensor_tensor(out=dsta, in0=ps[ab][:], in1=dsta, op=ALU.add), reads=[abk, ('Eacc', sl, half)], writes=[('Eacc', sl, half)])
                T.op('sp', lambda e: e.dma_start(out=ybuf[ex * cap:(ex + 1) * cap, :].rearrange("(s p) f -> p s f", p=128), in_=acc[:]),
                     reads=[('Eacc', sl, half) for sl in range(cap // 128) for half in range(2)], writes=[('ybuf', ex)], dsem='ybw')
            T.barrier()
        with contextlib.ExitStack() as es:
            def sb(name, shape, dtp):
                return es.enter_context(nc.sbuf_tensor(_PFX[0] + name, shape, dtp))
            y1 = [sb("Cy1_%d" % i, [128, D], F32) for i in range(2)]
            y2 = [sb("Cy2_%d" % i, [128, D], F32) for i in range(2)]
            xt = [sb("Cxt%d" % i, [128, D], F32) for i in range(2)]
            xo = [sb("Cxo%d" % i, [128, D], F32) for i in range(2)]
            if last:
                fg_ = sb("fgrow", [1, D], F32); FG = sb("FG", [128, D], F32)
                junk = sb("Cjunk", [128, D], BF16); ss = [sb("Css%d" % i, [128, 2], F32) for i in range(2)]
                nhalf = sb("Cnhalf", [128, 1], F32)
                T.op('pool', lambda e: e.memset(nhalf[:], -0.5), writes=['Cnhalf'])
                T.op('sp', lambda e: e.dma_start(out=fg_[:], in_=fng), writes=['fgrow'], dsem='c0')
                for half in range(2):
                    T.op('pe', lambda e: e.matmul(ps[half][:], lhsT=ones[0:1, :], rhs=fg_[0:1, half * 512:(half + 1) * 512], start=True, stop=True), reads=['ones', 'fgrow'], writes=[('ps', half)])
                    T.op('dve', lambda e: e.tensor_copy(out=FG[:, half * 512:(half + 1) * 512], in_=ps[half][:]), reads=[('ps', half)], writes=[('FG', half)])
            for t in range(ntiles):
                which = 0 if t < 32 else 1
                o = t % 2
                T.op('sp', lambda e: e.dma_start(out=xt[o][:], in_=X1d[t * 128:(t + 1) * 128, :]), writes=[('Cxt', o)], dsem='Cxt%d' % o)
                T.op('pool', lambda e: e.memset(y1[o][:], 0.0), writes=[('Cy1', o)])
                T.op('pool', lambda e: e.memset(y2[o][:], 0.0), writes=[('Cy2', o)])
                T.op('pool', lambda e: e.indirect_dma_start(out=y1[o][:, :], out_offset=None, in_=ybuf[:, :], in_offset=bass.IndirectOffsetOnAxis(ap=dest[:, 2 * t:2 * t + 1], axis=0),
                                                            bounds_check=bc_reg, oob_is_err=False), reads=['ybuf_all'], writes=[('Cy1', o)], dsem='Cy1_%d' % o)
                T.op('pool', lambda e: e.indirect_dma_start(out=y2[o][:, :], out_offset=None, in_=ybuf[:, :], in_offset=bass.IndirectOffsetOnAxis(ap=dest[:, 2 * t + 1:2 * t + 2], axis=0),
                                                            bounds_check=bc_reg, oob_is_err=False), reads=['ybuf_all'], writes=[('Cy2', o)], dsem='Cy2_%d' % o)
                T.op('dve', lambda e: e.tensor_scalar(out=y1[o][:], in0=y1[o][:], scalar1=gate[:, 2 * t:2 * t + 1], scalar2=None, op0=ALU.mult), reads=[('Cy1', o)], writes=[('Cy1', o)])
                T.op('dve', lambda e: e.scalar_tensor_tensor(out=y1[o][:], in0=y2[o][:], scalar=gate[:, 2 * t + 1:2 * t + 2], in1=y1[o][:], op0=ALU.mult, op1=ALU.add), reads=[('Cy1', o), ('Cy2', o)], writes=[('Cy1', o)])
                T.op('pool', lambda e: e.tensor_tensor(out=y1[o][:], in0=y1[o][:], in1=mods[(which, 5)][:], op=ALU.mult), reads=[('Cy1', o), ('mod', which, 5)], writes=[('Cy1', o)])
                T.op('pool', lambda e: e.tensor_tensor(out=xo[o][:], in0=y1[o][:], in1=xt[o][:], op=ALU.add), reads=[('Cy1', o), ('Cxt', o)], writes=[('Cxo', o)])
                if last:
                    T.op('act', lambda e: e.activation(out=junk[:], in_=xo[o][:], func=AF.Square, accum_out=ss[o][:, 0:1]), reads=[('Cxo', o)], writes=[('Css', o)])
                    T.op('dve', lambda e: e.tensor_scalar(out=ss[o][:, 1:2], in0=ss[o][:, 0:1], scalar1=1.0 / D, scalar2=EPS, op0=ALU.mult, op1=ALU.add), reads=[('Css', o)], writes=[('Css1', o)])
                    T.op('pool', lambda e: e.tensor_tensor(out=ss[o][:, 1:2], in0=ss[o][:, 1:2], in1=nhalf[:], op=ALU.pow), reads=[('Css1', o), 'Cnhalf'], writes=[('Css1', o)])
                    T.op('dve', lambda e: e.scalar_tensor_tensor(out=xo[o][:], in0=xo[o][:], scalar=ss[o][:, 1:2], in1=FG[:], op0=ALU.mult, op1=ALU.mult),
                         reads=[('Cxo', o), ('Css1', o), ('FG', 0), ('FG', 1)], writes=[('Cxo', o)])
                dst = yown[t * 128:(t + 1) * 128, :] if t < 32 else yc[(t - 32) * 128:(t - 31) * 128, :]
                T.op('sp', lambda e: e.dma_start(out=dst, in_=xo[o][:]), reads=[('Cxo', o)], writes=[('yo', t)], dsem='yout')


def attn_host_inputs(inp, i, x_lat, x_ctx, core, last, cap=CAP):
    j = i // 2
    b = core // 2; s = core % 2
    xw = np.zeros((NW * 128, D), np.float32)
    lo = s * 4096 - 128; hi = s * 4096 + 4096 + 128
    a = max(lo, 0); bb = min(hi, S)
    xw[a - lo: bb - lo] = x_lat[b, a:bb]
    wqkv = inp['attn_w_qkv'][j]
    wq = wqkv[:, :1024]; wk = wqkv[:, 1024:1280]; wv = wqkv[:, 1280:1536]
    d = np.arange(64)
    partner = np.where((d % 32) < 16, d + 16, d - 16)
    permq = (np.arange(16)[:, None] * 64 + partner[None, :]).reshape(-1)
    permk = (np.arange(4)[:, None] * 64 + partner[None, :]).reshape(-1)
    wqr = wq[:, permq]; wkr = wk[:, permk]
    wk2 = np.concatenate([wk.reshape(D, 4, 1, 64)] * 2, axis=2).reshape(D, 512)
    wk2r = np.concatenate([wkr.reshape(D, 4, 1, 64)] * 2, axis=2).reshape(D, 512)
    w = np.arange(NW * 128); t = s * 4096 - 128 + w
    valid = (t >= 0) & (t < S)
    tt = np.where(valid, t, 0)
    row = (tt // 64).astype(np.float32); col = (tt % 64).astype(np.float32)
    inv = (10000.0 ** (-np.arange(16, dtype=np.float32) / 16)).astype(np.float32)
    axis = d // 32; half = (d % 32) // 16; f = d % 16
    pos = np.where(axis[:, None] == 0, row[None, :], col[None, :]).astype(np.float32)
    ang = (pos * inv[f][:, None]).astype(np.float32)
    cosv = np.cos(ang).astype(np.float32); sinv = np.sin(ang).astype(np.float32)
    sins = np.where(half[:, None] == 0, -sinv, sinv)
    cost = np.ones((128, NTOK), np.float32); sint = np.zeros((128, NTOK), np.float32)
    cost[:, :NW * 128] = np.concatenate([cosv, cosv], 0); sint[:, :NW * 128] = np.concatenate([sins, sins], 0)
    jj = np.arange(128)[:, None]; qq = np.arange(128)[None, :]
    mP = (jj >= qq).astype(np.float32); mN = (jj <= qq).astype(np.float32)
    vP = 1.0 if s == 1 else 0.0; vN = 1.0 if s == 0 else 0.0
    masks = np.concatenate([mP, mP * vP, mN, mN * vN], axis=1).astype(np.float32)
    cvec = np.concatenate([inp['c'][b].reshape(8, 128).T, inp['c_ctx'].reshape(8, 128).T], axis=1).astype(np.float32)
    return dict(xwin=xw, xc=np.ascontiguousarray(x_ctx[b]), cvec=np.ascontiguousarray(cvec), ada_w=inp['ada_w'][i], ada_b=inp['ada_b'][i][None, :],
                nmg=inp['norm_mix_g'][i][None, :], nfg=inp['norm_ffn_g'][i][None, :],
                wq=np.ascontiguousarray(wq), wqr=np.ascontiguousarray(wqr), wk2=np.ascontiguousarray(wk2), wk2r=np.ascontiguousarray(wk2r),
                wv=np.ascontiguousarray(wv), wo=inp['attn_w_o'][j], cost=cost, sint=sint, sinks=inp['attn_sinks'][j][None, :], masks=masks,
                wr=inp['moe_w_router'][j], mwg=inp['moe_w_gate'][j], mwu=inp['moe_w_up'][j], mwd=inp['moe_w_down'][j],
                fng=inp['final_norm_g'][None, :], ident=np.eye(128, dtype=np.float32),
                ut=np.triu(np.ones((128, 128), np.float32), 1), ebase=np.tile((np.arange(8) * cap).astype(np.float32)[None, :], (128, 1)))


_FNET_W = ['cvec', 'ada_w', 'ada_b', 'nmg', 'nfg', 'w_out', 'wg', 'wu', 'wd']
_ATTN_W = ['cvec', 'ada_w', 'ada_b', 'nmg', 'nfg', 'wq', 'wqr', 'wk2', 'wk2r', 'wv', 'wo', 'sinks', 'wr', 'mwg', 'mwu', 'mwd']
_SHAPES = dict(cvec=[128, 16], ada_w=[D, 6 * D], ada_b=[1, 6 * D], nmg=[1, D], nfg=[1, D], w_out=[D, D], wg=[D, DFF], wu=[D, DFF], wd=[DFF, D],
               wq=[D, D], wqr=[D, D], wk2=[D, 512], wk2r=[D, 512], wv=[D, 256], wo=[D, D], sinks=[1, 16], wr=[D, 8],
               mwg=[8, D, DFF], mwu=[8, D, DFF], mwd=[8, DFF, D])
_SHARED = dict(cs256=[256, 512], twc=[128, 64 * 128], tws=[128, 64 * 128], w64_0=[128, 32], w64_1=[128, 32],
               cost_0=[128, NTOK], sint_0=[128, NTOK], masks_0=[128, 512], cost_1=[128, NTOK], sint_1=[128, NTOK], masks_1=[128, 512],
               fng=[1, D], ut=[128, 128], ebase=[128, 8])


def build_fused(cap=CAP):
    nc = bass.Bass("TRN2", target_bir_lowering=False)

    def dt(name, shape, dtype=F32, kind="ExternalInput"):
        return nc.dram_tensor(name, shape, dtype, kind=kind).ap()
    x = dt("x", [S, D]); ctx = dt("ctx", [256, D]); ident = dt("ident", [128, 128])
    out = dt("out", [4096, D], kind="ExternalOutput")
    shared = {k: dt(k, shp) for k, shp in _SHARED.items()}
    LW = []
    for i in range(4):
        names = _FNET_W if i % 2 == 0 else _ATTN_W
        w = {k: dt("L%d_%s" % (i, k), _SHAPES[k]) for k in names}
        w.update(shared)
        LW.append(w)
    Y = [dt("Y%d" % i, [S, D], F32, kind="Internal") for i in range(3)]
    C = [dt("C%d" % i, [256, D], F32, kind="Internal") for i in range(3)]
    SC = dict(ABd=dt("ABd", [S, 2048], BF16, kind="Internal"), Gd=dt("Gd", [64, 128, 2, 1024], BF16, kind="Internal"),
              X1d=dt("X1d", [4352, D], F32, kind="Internal"),
              wgb=dt("wgb", [D, DFF], BF16, kind="Internal"), wub=dt("wub", [D, DFF], BF16, kind="Internal"),
              wdb=dt("wdb", [DFF, D], BF16, kind="Internal"), woutb=dt("woutb", [D, D], BF16, kind="Internal"),
              hTd=dt("hTd", [128, 8, NTOK], BF16, kind="Internal"), qTd=dt("qTd", [128, 8, 4352], BF16, kind="Internal"),
              modc=dt("modc", [12, 128, D], F32, kind="Internal"), hbuf=dt("hbuf", [8 * cap, D], BF16, kind="Internal"), ybuf=dt("ybuf", [8 * cap, D], F32, kind="Internal"))
    with contextlib.ExitStack() as es:
        T = Tr(nc, es)

        def sbg(name, shape, dtp):
            return es.enter_context(nc.sbuf_tensor(name, shape, dtp))
        ps = [es.enter_context(nc.psum_tensor("bank%d" % i, [128, 512], F32)) for i in range(8)]
        common = setup_common(nc, T, es, sbg, ps, ident)
        T.barrier()
        for i in range(4):
            A = dict(xin=x if i == 0 else Y[i - 1], cin=ctx if i == 0 else C[i - 1],
                     yout=out if i == 3 else Y[i], cout=None if i == 3 else C[i])
            for s in range(1 if i == 3 else 2):
                _PFX[0] = "L%ds%d_" % (i, s)
                if i % 2 == 0:
                    emit_fnet(nc, T, ps, common, LW[i], A, SC, s, first_pass=(s == 0), lat_tiles=([0, 31] if (i == 2 and s == 1) else None))
                else:
                    emit_attn(nc, T, ps, common, LW[i], A, SC, s, do_ctx=(s == 0 and i != 3), final=(i == 3), cap=cap, mod_load=(s == 1))
                T.barrier()
    return nc


def attn_tables(s):
    d = np.arange(64)
    w = np.arange(NW * 128); t = s * 4096 - 128 + w
    valid = (t >= 0) & (t < S)
    tt = np.where(valid, t, 0)
    row = (tt // 64).astype(np.float32); col = (tt % 64).astype(np.float32)
    inv = (10000.0 ** (-np.arange(16, dtype=np.float32) / 16)).astype(np.float32)
    axis = d // 32; half = (d % 32) // 16; f = d % 16
    pos = np.where(axis[:, None] == 0, row[None, :], col[None, :]).astype(np.float32)
    ang = (pos * inv[f][:, None]).astype(np.float32)
    cosv = np.cos(ang).astype(np.float32); sinv = np.sin(ang).astype(np.float32)
    sins = np.where(half[:, None] == 0, -sinv, sinv)
    cost = np.ones((128, NTOK), np.float32); sint = np.zeros((128, NTOK), np.float32)
    cost[:, :NW * 128] = np.concatenate([cosv, cosv], 0); sint[:, :NW * 128] = np.concatenate([sins, sins], 0)
    jj = np.arange(128)[:, None]; qq = np.arange(128)[None, :]
    mP = (jj >= qq).astype(np.float32); mN = (jj <= qq).astype(np.float32)
    vP = 1.0 if s == 1 else 0.0; vN = 1.0 if s == 0 else 0.0
    masks = np.concatenate([mP, mP * vP, mN, mN * vN], axis=1).astype(np.float32)
    return cost, sint, masks


def host_inputs(inp, b, swapped, cap=CAP):
    xb = inp['x'][b]
    if swapped:
        xb = np.concatenate([xb[4096:], xb[:4096]], axis=0)
    m = dict(x=np.ascontiguousarray(xb), ctx=np.ascontiguousarray(inp['ctx'][b]), ident=np.eye(128, dtype=np.float32))
    rs = (lambda s: 1 - s) if swapped else (lambda s: s)
    cs256, twc, tws, w64_0 = fnet_tables(rs(0), shift=4096 if swapped else 0)
    _, _, _, w64_1 = fnet_tables(rs(1), shift=4096 if swapped else 0)
    m.update(cs256=cs256, twc=twc, tws=tws, w64_0=w64_0, w64_1=w64_1)
    for s in range(2):
        cost, sint, masks = attn_tables(rs(s))
        m['cost_%d' % s] = cost; m['sint_%d' % s] = sint; m['masks_%d' % s] = masks
    m['fng'] = inp['final_norm_g'][None, :]
    m['ut'] = np.triu(np.ones((128, 128), np.float32), 1)
    m['ebase'] = np.tile((np.arange(8) * cap).astype(np.float32)[None, :], (128, 1))
    cvec = np.ascontiguousarray(np.concatenate([inp['c'][b].reshape(8, 128).T, inp['c_ctx'].reshape(8, 128).T], axis=1).astype(np.float32))
    d = np.arange(64)
    partner = np.where((d % 32) < 16, d + 16, d - 16)
    permq = (np.arange(16)[:, None] * 64 + partner[None, :]).reshape(-1)
    permk = (np.arange(4)[:, None] * 64 + partner[None, :]).reshape(-1)
    for i in range(4):
        j = i // 2
        w = dict(cvec=cvec, ada_w=inp['ada_w'][i], ada_b=inp['ada_b'][i][None, :], nmg=inp['norm_mix_g'][i][None, :], nfg=inp['norm_ffn_g'][i][None, :])
        if i % 2 == 0:
            w.update(w_out=inp['fnet_w_out'][j], wg=inp['ffn_w_gate'][j], wu=inp['ffn_w_up'][j], wd=inp['ffn_w_down'][j])
        else:
            wqkv = inp['attn_w_qkv'][j]
            wq = wqkv[:, :1024]; wk = wqkv[:, 1024:1280]; wv = wqkv[:, 1280:1536]
            wqr = wq[:, permq]; wkr = wk[:, permk]
            wk2 = np.concatenate([wk.reshape(D, 4, 1, 64)] * 2, axis=2).reshape(D, 512)
            wk2r = np.concatenate([wkr.reshape(D, 4, 1, 64)] * 2, axis=2).reshape(D, 512)
            w.update(wq=wq, wqr=wqr, wk2=wk2, wk2r=wk2r, wv=wv, wo=inp['attn_w_o'][j], sinks=inp['attn_sinks'][j][None, :],
                     wr=inp['moe_w_router'][j], mwg=inp['moe_w_gate'][j], mwu=inp['moe_w_up'][j], mwd=inp['moe_w_down'][j])
        for k, v in w.items():
            m['L%d_%s' % (i, k)] = np.ascontiguousarray(v, dtype=np.float32)
    return m


_PROG = {}


def kernel(**inputs):
    inp = {k: np.ascontiguousarray(np.asarray(v, dtype=np.float32)) for k, v in inputs.items()}
    if 'nc' not in _PROG:
        _PROG['nc'] = build_fused()
    nc = _PROG['nc']
    in_maps = [host_inputs(inp, c % 4, swapped=(c >= 4)) for c in range(8)]
    res = run_bass_kernel_spmd(nc, in_maps, core_ids=list(range(8)))
    return np.ascontiguousarray(np.stack([np.concatenate([res.results[b]['out'], res.results[b + 4]['out']], axis=0) for b in range(4)]).astype(np.float32))
```

```python
import numpy as np
import contextlib
import concourse.bass as bass
import concourse.mybir as mybir
from concourse.bass_utils import run_bass_kernel_spmd

F32 = mybir.dt.float32
BF16 = mybir.dt.bfloat16
I32 = mybir.dt.int32
U32 = mybir.dt.uint32
AF = mybir.ActivationFunctionType
ALU = mybir.AluOpType
AX = mybir.AxisListType

D = 1024
S = 8192
LCTX = 256
DFF = 3584
NF = 28
EPS = 1e-6


class Tr:
    def __init__(self, nc, es):
        self.nc = nc
        self.es = es
        self.eng = {'pe': nc.tensor, 'act': nc.scalar, 'dve': nc.vector, 'pool': nc.gpsimd, 'sp': nc.sync}
        self.esem = {}
        self.ecnt = {}
        self.known = {k: {} for k in self.eng}
        self.state = {}
        self.dsem = {}
        self.dtot = {}
        for k in ['pe', 'act', 'dve', 'pool']:
            self.esem[k] = es.enter_context(nc.semaphore("es_" + k))
            self.ecnt[k] = 0
        self.nwaits = 0
        self.nops = 0
        self.uq_free = []
        self.uq_used = []
        self.uq_n = 0

    def dma_sem(self, name):
        if name not in self.dsem:
            s = self.es.enter_context(self.nc.semaphore("ds_" + name))
            self.dsem[name] = [s, 0]
            self.dtot[id(s)] = self.dsem[name]
        return self.dsem[name]

    def _need(self, e, ev):
        sem, val, src = ev
        if src == e and e == 'pe':
            return
        k = self.known[e]
        if src == 'dma':
            val = max(val, self.dtot[id(sem)][1])
        if k.get(id(sem), 0) >= val:
            return
        self.eng[e].wait_ge(sem, val)
        self.nwaits += 1
        k[id(sem)] = val

    def op(self, e, fn, reads=(), writes=(), dsem=None):
        for key in reads:
            st = self.state.get(key)
            if st:
                for ev in st['w']:
                    self._need(e, ev)
        for key in writes:
            st = self.state.get(key)
            if st:
                for ev in st['w']:
                    self._need(e, ev)
                for ev in st['r']:
                    self._need(e, ev)
        ins = fn(self.eng[e])
        self.nops += 1
        if dsem is not None:
            if dsem in ('c0', 'c1', 'uniq'):
                if not self.uq_free:
                    self.uq_n += 1
                    self.uq_free.append('uq%d' % self.uq_n)
                dsem = self.uq_free.pop()
                self.uq_used.append(dsem)
            d = self.dma_sem(dsem)
            d[1] += 16
            ins.then_inc(d[0], 16)
            ev = (d[0], d[1], 'dma')
        else:
            self.ecnt[e] += 1
            ins.then_inc(self.esem[e], 1)
            ev = (self.esem[e], self.ecnt[e], e)
        for key in reads:
            st = self.state.setdefault(key, {'w': [], 'r': []})
            st['r'].append(ev)
            if len(st['r']) > 64:
                best = {}
                for s_, v_, src_ in st['r']:
                    if id(s_) not in best or best[id(s_)][1] < v_:
                        best[id(s_)] = (s_, v_, src_)
                st['r'] = list(best.values())
        for key in writes:
            self.state[key] = {'w': [ev], 'r': []}
        return ev

    def barrier(self, engines=('pe', 'act', 'dve', 'pool', 'sp')):
        for e in engines:
            for k in ['pe', 'act', 'dve', 'pool']:
                if self.ecnt[k] > 0:
                    self._need(e, (self.esem[k], self.ecnt[k], k if k != 'pe' else 'x'))
            for name, (s, c) in self.dsem.items():
                if c > 0:
                    self._need(e, (s, c, 'dma'))
        self.state = {}
        self.uq_free.extend(self.uq_used)
        self.uq_used = []


class Ring:
    def __init__(self, n):
        self.n = n
        self.i = -1

    def next(self):
        self.i = (self.i + 1) % self.n
        return self.i


_PFX = ['']


def setup_common(nc, T, es, sb, ps_banks, ident_d):
    idf = sb("idf", [128, 128], F32)
    idb = sb("idb", [128, 128], BF16)
    ones = sb("ones", [128, 128], F32)
    T.op('sp', lambda e: e.dma_start(out=idf[:], in_=ident_d), writes=['idf'], dsem='c0')
    T.op('dve', lambda e: e.tensor_copy(out=idb[:], in_=idf[:]), reads=['idf'], writes=['idb'])
    T.op('pool', lambda e: e.memset(ones[:], 1.0), writes=['ones'])
    return idf, idb, ones


def compute_mod(nc, T, sb_tmp, ps, cvec, ada_w, ada_b, ones, mods, gains, cache=None, load=False):
    if load:
        for (which, idx), tile in mods.items():
            T.op('sp', lambda e: e.dma_start(out=tile[:], in_=cache[which * 6 + idx]), writes=[('mod', which, idx)], dsem='uniq')
        T.barrier()
        return
    with contextlib.ExitStack() as es2:
        def sb(name, shape, dt):
            return es2.enter_context(nc.sbuf_tensor(_PFX[0] + name, shape, dt))
        cv = sb("cv", [128, 16], F32)
        sc = sb("scv", [128, 16], F32)
        screp = sb("screp", [128, 16, 128], F32)
        brow = sb("brow", [1, 6 * D], F32)
        grow = sb("grow", [1, 2 * D], F32)
        aw = [sb("aw%d" % i, [128, 8, 512], F32) for i in range(2)]
        T.op('sp', lambda e: e.dma_start(out=cv[:], in_=cvec), writes=['cv'], dsem='c0')
        T.op('sp', lambda e: e.dma_start(out=brow[:], in_=ada_b), writes=['brow'], dsem='c0')
        for gi, (gd, _) in enumerate(gains):
            T.op('sp', lambda e: e.dma_start(out=grow[:, gi * D:(gi + 1) * D], in_=gd), writes=[('grow', gi)], dsem='c0')
        T.op('act', lambda e: e.activation(out=sc[:], in_=cv[:], func=AF.Silu), reads=['cv'], writes=['sc'])
        for j in range(16):
            T.op('dve', lambda e: e.tensor_scalar(out=screp[:, j, :], in0=ones[:], scalar1=sc[:, j:j + 1], scalar2=None, op0=ALU.mult),
                 reads=['sc', 'ones'], writes=[('screp', j)])
        awv = ada_w.rearrange("(c p) f -> p c f", p=128)
        for n in range(12):
            a = aw[n % 2]
            ak = ('aw', n % 2)
            T.op('sp', lambda e: e.dma_start(out=a[:], in_=awv[:, :, n * 512:(n + 1) * 512]), writes=[ak], dsem='aw%d' % (n % 2))
            for which in range(2):
                bank = ps[which]
                bk = ('ps', which)
                for kc in range(8):
                    T.op('pe', lambda e: e.matmul(bank[:], lhsT=screp[:, which * 8 + kc, :], rhs=a[:, kc, :], start=(kc == 0), stop=False),
                         reads=[('screp', which * 8 + kc), ak], writes=[bk])
                T.op('pe', lambda e: e.matmul(bank[:], lhsT=ones[0:1, :], rhs=brow[0:1, n * 512:(n + 1) * 512], start=False, stop=True),
                     reads=['ones', 'brow'], writes=[bk])
                idx = n // 2
                dst = mods[(which, idx)]
                half = n % 2
                T.op('dve' if which == 0 else 'act',
                     (lambda e: e.tensor_copy(out=dst[:, half * 512:(half + 1) * 512], in_=bank[:])) if which == 0 else
                     (lambda e: e.activation(out=dst[:, half * 512:(half + 1) * 512], in_=bank[:], func=AF.Copy)),
                     reads=[bk], writes=[('mod', which, idx)])
        for gi, (gd, lst) in enumerate(gains):
            for half in range(2):
                bank = ps[2 + half]
                bk = ('ps', 2 + half)
                T.op('pe', lambda e: e.matmul(bank[:], lhsT=ones[0:1, :], rhs=grow[0:1, gi * D + half * 512: gi * D + (half + 1) * 512], start=True, stop=True),
                     reads=['ones', ('grow', gi)], writes=[bk])
                for (which, idx) in lst:
                    dst = mods[(which, idx)]
                    T.op('dve', lambda e: e.scalar_tensor_tensor(out=dst[:, half * 512:(half + 1) * 512], in0=dst[:, half * 512:(half + 1) * 512],
                                                                 scalar=1.0, in1=bank[:], op0=ALU.add, op1=ALU.mult),
                         reads=[bk, ('mod', which, idx)], writes=[('mod', which, idx)])
        if cache is not None:
            for (which, idx), tile in mods.items():
                T.op('sp', lambda e: e.dma_start(out=cache[which * 6 + idx], in_=tile[:]), reads=[('mod', which, idx)], writes=[('modc', which, idx)], dsem='modw')
        T.barrier()


class NormCtx:
    def __init__(self, nc, T, sb, idb, pfx="", nbuf=2):
        self.nc = nc
        self.T = T
        self.idb = idb
        self.pfx = pfx
        self.junk = sb(pfx + "junk", [128, 1024], BF16)
        self.ss = [sb(pfx + "ss%d" % i, [128, 2], F32) for i in range(nbuf)]
        self.h1 = [sb(pfx + "h1_%d" % i, [128, 1024], F32) for i in range(nbuf)]
        self.hb = [sb(pfx + "hb_%d" % i, [128, 1024], BF16) for i in range(nbuf)]
        self.r = Ring(nbuf)
        self.nhalf = sb(pfx + "nhalf", [128, 1], F32)
        T.op('pool', lambda e: e.memset(self.nhalf[:], -0.5), writes=[pfx + 'nhalf'])

    def run(self, xt, xkey, GS, gskey, SH, shkey, psT, pskey, out_ap, outkey, evac_eng='act'):
        i = self.pre(xt, xkey, GS, gskey, SH, shkey)
        self.post(i, psT, pskey, out_ap, outkey, evac_eng)

    def pre(self, xt, xkey, GS, gskey, SH, shkey):
        T = self.T
        i = self.r.next()
        p = self.pfx
        ss, h1, hb = self.ss[i], self.h1[i], self.hb[i]
        T.op('act', lambda e: e.activation(out=self.junk[:], in_=xt, func=AF.Square, accum_out=ss[:, 0:1]), reads=[xkey], writes=[(p + 'ss', i)])
        T.op('dve', lambda e: e.tensor_scalar(out=ss[:, 1:2], in0=ss[:, 0:1], scalar1=1.0 / D, scalar2=EPS, op0=ALU.mult, op1=ALU.add),
             reads=[(p + 'ss', i)], writes=[(p + 'ss1', i)])
        T.op('pool', lambda e: e.tensor_tensor(out=ss[:, 1:2], in0=ss[:, 1:2], in1=self.nhalf[:], op=ALU.pow),
             reads=[(p + 'ss1', i), p + 'nhalf'], writes=[(p + 'ss1', i)])
        T.op('dve', lambda e: e.scalar_tensor_tensor(out=h1[:], in0=xt, scalar=ss[:, 1:2], in1=GS, op0=ALU.mult, op1=ALU.mult),
             reads=[xkey, (p + 'ss1', i), gskey], writes=[(p + 'h1', i)])
        T.op('pool', lambda e: e.tensor_tensor(out=hb[:], in0=h1[:], in1=SH, op=ALU.add), reads=[(p + 'h1', i), shkey], writes=[(p + 'hb', i)])
        return i

    def post(self, i, psT, pskey, out_ap, outkey, evac_eng='act'):
        T = self.T
        p = self.pfx
        hb = self.hb[i]
        pT = psT[:].bitcast(BF16).rearrange("p (c t) -> p c t", c=8)
        for c in range(8):
            T.op('pe', lambda e: e.transpose(out=pT[:, c, :], in_=hb[:, c * 128:(c + 1) * 128], identity=self.idb[:]),
                 reads=[(p + 'hb', i), 'idb'], writes=[pskey])
        if evac_eng == 'act':
            T.op('act', lambda e: e.activation(out=out_ap, in_=pT, func=AF.Copy), reads=[pskey], writes=[outkey])
        else:
            T.op('dve', lambda e: e.tensor_copy(out=out_ap, in_=pT), reads=[pskey], writes=[outkey])


def emit_fnet(nc, T, ps, common, W, A, SC, s, first_pass, lat_tiles=None):
    debug_stop = None
    idf, idb, ones = common
    xfull = A['xin']; xc = A['cin']; xown = xfull[s * 4096:(s + 1) * 4096, :]
    yown = A['yout'][s * 4096:(s + 1) * 4096, :]; yc = A['cout']
    cvec = W['cvec']; ada_w = W['ada_w']; ada_b = W['ada_b']; nmg = W['nmg']; nfg = W['nfg']
    w_out = W['w_out']; wg = W['wg']; wu = W['wu']; wd = W['wd']
    cs256 = W['cs256']; twc = W['twc']; tws = W['tws']; w64 = W['w64_%d' % s]
    ABd = SC['ABd']; Gd = SC['Gd']; X1d = SC['X1d']; wgb = SC['wgb']; wub = SC['wub']; wdb = SC['wdb']; woutb = SC['woutb']
    NA = 1 if first_pass else 0
    if lat_tiles is None:
        lat_tiles = list(range(32))

    with contextlib.ExitStack() as es:
        def sbg(name, shape, dtp):
            return es.enter_context(nc.sbuf_tensor(_PFX[0] + name, shape, dtp))
        if first_pass:
          T.op('pool', lambda e: e.dma_start(out=wgb.rearrange("k (a b) -> (k a) b", b=1792), in_=wg.rearrange("k (a b) -> (k a) b", b=1792)), writes=['wgb'], dsem='wconv')
          T.op('pool', lambda e: e.dma_start(out=wub.rearrange("k (a b) -> (k a) b", b=1792), in_=wu.rearrange("k (a b) -> (k a) b", b=1792)), writes=['wub'], dsem='wconv')
          T.op('pool', lambda e: e.dma_start(out=wdb, in_=wd), writes=['wdb'], dsem='wconv')
          T.op('pool', lambda e: e.dma_start(out=woutb, in_=w_out), writes=['woutb'], dsem='wconv')
        mods = {}
        for which in range(2):
            for idx in (3, 4, 5):
                mods[(which, idx)] = sbg("mod%d_%d" % (which, idx), [128, D], F32)
        with contextlib.ExitStack() as es_mix:
            def sbm(name, shape, dtp):
                return es_mix.enter_context(nc.sbuf_tensor(_PFX[0] + name, shape, dtp))
            for which in range(2):
                for idx in (0, 1, 2):
                    mods[(which, idx)] = sbm("mod%d_%d" % (which, idx), [128, D], F32)
            compute_mod(nc, T, None, ps, cvec, ada_w, ada_b, ones, mods,
                        [(nmg, [(0, 1), (1, 1)]), (nfg, [(0, 4), (1, 4)])], cache=SC['modc'], load=(not first_pass))
            YT = sbm("YT", [128, 8, 4096], BF16)
            YTc = sbm("YTc", [128, 8, 256], BF16)
            CS = sbm("CS", [128, 2, 512], BF16)
            T.op('pool', lambda e: e.dma_start(out=CS[:], in_=cs256.rearrange("(k p) f -> p k f", p=128)), writes=['CS'], dsem='c1')

            with contextlib.ExitStack() as esA:
                def sba(name, shape, dtp):
                    return esA.enter_context(nc.sbuf_tensor(_PFX[0] + name, shape, dtp))
                NC = NormCtx(nc, T, sba, idb, "A", nbuf=4)
                xt = [sba("Axt%d" % i, [128, D], F32) for i in range(4)]
                hT = [sba("AhT%d" % i, [128, 8, 128], BF16) for i in range(3)]
                ABt = [sba("ABt%d" % i, [128, 2048], BF16) for i in range(3)]
                ABc = sba("ABc", [128, 2, 2048], BF16)
                Bnc = sba("Bnc", [128, 2, 1024], BF16)
                NTA = (64 + 2) * NA
                xr = Ring(4); hr = Ring(3); ar = Ring(3); dbank = [0]

                def src_tile(t):
                    return xfull[t * 128:(t + 1) * 128, :] if t < 64 else xc[(t - 64) * 128:(t - 63) * 128, :]
                loaded = {}

                def issue_load(t):
                    if t >= NTA:
                        return
                    i = xr.next()
                    T.op('sp', lambda e: e.dma_start(out=xt[i][:], in_=src_tile(t)), writes=[('Axt', i)], dsem='Axt%d' % i)
                    loaded[t] = i
                issue_load(0); issue_load(1); issue_load(2)
                for t in range(NTA):
                    issue_load(t + 3)
                    i = loaded[t]
                    which = 0 if t < 64 else 1
                    hi = hr.next()
                    NC.run(xt[i][:], ('Axt', i), mods[(which, 1)][:], ('mod', which, 1), mods[(which, 0)][:], ('mod', which, 0),
                           ps[6 + (t % 2)], ('ps', 6 + (t % 2)), hT[hi][:], ('AhT', hi), evac_eng='act')
                    ai = ar.next()
                    for g in range(4):
                        gb = dbank[0] % 6; dbank[0] += 1
                        for kk in range(2):
                            T.op('pe', lambda e: e.matmul(ps[gb][:], lhsT=hT[hi][:, 2 * g + kk, :], rhs=CS[:, kk, :], start=(kk == 0), stop=(kk == 1)),
                                 reads=[('AhT', hi), 'CS'], writes=[('ps', gb)])
                        if t < 64:
                            dst = ABt[ai][:, g * 512:(g + 1) * 512]; dk = ('ABt', ai, g)
                        else:
                            dst = ABc[:, t - 64, g * 512:(g + 1) * 512]; dk = ('ABc', t - 64, g)
                        if g % 2 == 0:
                            T.op('dve', lambda e: e.tensor_copy(out=dst, in_=ps[gb][:]), reads=[('ps', gb)], writes=[dk])
                        else:
                            T.op('act', lambda e: e.activation(out=dst, in_=ps[gb][:], func=AF.Copy), reads=[('ps', gb)], writes=[dk])
                    if t < 64:
                        T.op('sp', lambda e: e.dma_start(out=ABd[t * 128:(t + 1) * 128, :], in_=ABt[ai][:]),
                             reads=[('ABt', ai, g) for g in range(4)], writes=[('ABd', t)], dsem='abw')
                for tt in range(2 * NA):
                    T.op('pool', lambda e: e.tensor_scalar(out=Bnc[:, tt, :].rearrange("p (g x) -> p g x", g=4),
                                                           in0=ABc[:, tt, :].rearrange("p (g x) -> p g x", g=4)[:, :, 256:512],
                                                           scalar1=-1.0, scalar2=None, op0=ALU.mult),
                         reads=[('ABc', tt, g) for g in range(4)], writes=[('Bnc', tt)])
                for c in range(8 * NA):
                    g = c // 2
                    bank = ps[c // 2]
                    o = bank[:, (c % 2) * 256:(c % 2) * 256 + 256]
                    n_mm = 0
                    for nt in range(2):
                        T.op('pe', lambda e: e.matmul(o, lhsT=ABc[:, nt, g * 512 + (c % 2) * 128: g * 512 + (c % 2) * 128 + 128], rhs=CS[:, nt, 0:256], start=(nt == 0), stop=False),
                             reads=[('ABc', nt, g), 'CS'], writes=[('ps', c // 2)])
                        T.op('pe', lambda e: e.matmul(o, lhsT=Bnc[:, nt, g * 256 + (c % 2) * 128: g * 256 + (c % 2) * 128 + 128], rhs=CS[:, nt, 256:512], start=False, stop=(nt == 1)),
                             reads=[('Bnc', nt), 'CS'], writes=[('ps', c // 2)])
                    if c % 2 == 1:
                        T.op('act', lambda e: e.activation(out=YTc[:, c - 1:c + 1, :], in_=bank[:].rearrange("p (c k) -> p c k", c=2), func=AF.Identity, scale=1.0 / 256.0),
                             reads=[('ps', c // 2)], writes=[('YTc', c // 2)])
                T.barrier()
            if debug_stop == 'A':
                return nc
            with contextlib.ExitStack() as esS:
                def sbs(name, shape, dtp):
                    return esS.enter_context(nc.sbuf_tensor(_PFX[0] + name, shape, dtp))
                TWC = sbs("TWC", [128, 64, 128], BF16); TWS = sbs("TWS", [128, 64, 128], BF16)
                for q in range(4 * NA):
                    T.op('pool', lambda e: e.dma_start(out=TWC[:, q * 16:(q + 1) * 16, :], in_=twc.rearrange("p (j k) -> p j k", k=128)[:, q * 16:(q + 1) * 16, :]), writes=[('TWC', q)], dsem='c1')
                    T.op('pool', lambda e: e.dma_start(out=TWS[:, q * 16:(q + 1) * 16, :], in_=tws.rearrange("p (j k) -> p j k", k=128)[:, q * 16:(q + 1) * 16, :]), writes=[('TWS', q)], dsem='c1')
                Z = [sbs("Z%d" % i, [128, 4, 2048], BF16) for i in range(2)]
                Bn = [sbs("Bn%d" % i, [128, 1024], BF16) for i in range(2)]
                Gt = [sbs("Gt%d" % i, [128, 2, 1024], BF16) for i in range(2)]
                ABv = ABd.rearrange("(m j) c -> m j c", j=64)
                abkeys = [('ABd', t) for t in range(64)]
                zload = {}

                def issue_z(jg):
                    if jg >= 16 * NA:
                        return
                    i = jg % 2
                    T.op('sp', lambda e: e.dma_start(out=Z[i][:], in_=ABv[:, jg * 4:(jg + 1) * 4, :]), reads=abkeys, writes=[('Z', i)], dsem='Z%d' % i)
                issue_z(0)
                sc1 = 1.0 / np.sqrt(8192.0 * 256.0)
                for jg in range(16 * NA):
                    issue_z(jg + 1)
                    zi = jg % 2
                    for jj in range(4):
                        j = jg * 4 + jj
                        q = j // 16
                        bi = j % 2
                        Zv = Z[zi][:, jj, :].rearrange("p (g x) -> p g x", g=4)
                        A = Zv[:, :, 0:256]; B = Zv[:, :, 256:512]
                        Bnv = Bn[bi][:].rearrange("p (g x) -> p g x", g=4)
                        T.op('act', lambda e: e.activation(out=Bnv, in_=B, func=AF.Identity, scale=-1.0), reads=[('Z', zi)], writes=[('Bn', bi)])
                        for h in range(2):
                            br = ps[(j % 2) * 4 + h * 2]; bkr = ('ps', (j % 2) * 4 + h * 2)
                            bim = ps[(j % 2) * 4 + h * 2 + 1]; bki = ('ps', (j % 2) * 4 + h * 2 + 1)
                            T.op('pe', lambda e: e.matmul(br[:], lhsT=TWC[:, j, :], rhs=A[:, 2 * h:2 * h + 2, :], start=True, stop=False), reads=[('TWC', q), ('Z', zi)], writes=[bkr])
                            T.op('pe', lambda e: e.matmul(br[:], lhsT=TWS[:, j, :], rhs=Bnv[:, 2 * h:2 * h + 2, :], start=False, stop=True), reads=[('TWS', q), ('Bn', bi)], writes=[bkr])
                            T.op('pe', lambda e: e.matmul(bim[:], lhsT=TWS[:, j, :], rhs=A[:, 2 * h:2 * h + 2, :], start=True, stop=False), reads=[('TWS', q), ('Z', zi)], writes=[bki])
                            T.op('pe', lambda e: e.matmul(bim[:], lhsT=TWC[:, j, :], rhs=B[:, 2 * h:2 * h + 2, :], start=False, stop=True), reads=[('TWC', q), ('Z', zi)], writes=[bki])
                            T.op('act', lambda e: e.activation(out=Gt[bi][:, 0, h * 512:(h + 1) * 512], in_=br[:], func=AF.Identity, scale=sc1), reads=[bkr], writes=[('Gt', bi, 0, h)])
                            T.op('dve', lambda e: e.tensor_scalar(out=Gt[bi][:, 1, h * 512:(h + 1) * 512], in0=bim[:], scalar1=sc1, scalar2=None, op0=ALU.mult), reads=[bki], writes=[('Gt', bi, 1, h)])
                        T.op('sp', lambda e: e.dma_start(out=Gd[j], in_=Gt[bi][:]), reads=[('Gt', bi, r, h) for r in range(2) for h in range(2)], writes=[('Gd', j)], dsem='gw')
                T.barrier()
            if debug_stop == 'S1':
                return nc
            with contextlib.ExitStack() as es2:
                def sb2(name, shape, dtp):
                    return es2.enter_context(nc.sbuf_tensor(_PFX[0] + name, shape, dtp))
                W64f = sb2("W64f", [128, 32], F32); W64 = sb2("W64", [128, 32], BF16)
                T.op('sp', lambda e: e.dma_start(out=W64f[:], in_=w64), writes=['W64f'], dsem='c0')
                T.op('dve', lambda e: e.tensor_copy(out=W64[:], in_=W64f[:]), reads=['W64f'], writes=['W64'])
                G2 = [sb2("G2_%d" % i, [128, 4, 1024], BF16) for i in range(3)]
                gkeys = [('Gd', j) for j in range(64)]
                YTv = YT[:].rearrange("p c (a b) -> p c a b", b=128)

                def issue_g(kg):
                    if kg >= 32:
                        return
                    i = kg % 3
                    for ri in range(2):
                        T.op('sp', lambda e: e.dma_start(out=G2[i][ri * 64:(ri + 1) * 64, :, :], in_=Gd[:, kg * 4:(kg + 1) * 4, ri, :]), reads=gkeys, writes=[('G2', i, ri)], dsem='G2_%d' % i)
                issue_g(0); issue_g(1)
                for kg in range(32):
                    issue_g(kg + 2)
                    i = kg % 3
                    for cb in range(2):
                        bank = ps[(kg % 2) * 2 + cb]; bk = ('ps', (kg % 2) * 2 + cb)
                        bv = bank[:].rearrange("p (c b a) -> p c b a", c=4, b=4)
                        for kbi in range(4):
                            for cc in range(4):
                                c = cb * 4 + cc
                                T.op('pe', lambda e: e.matmul(bv[:, cc, kbi, :], lhsT=G2[i][:, kbi, c * 128:(c + 1) * 128], rhs=W64[:], start=True, stop=True),
                                     reads=[('G2', i, 0), ('G2', i, 1), 'W64'], writes=[bk])
                        src = bank[:].rearrange("p (c b a) -> p c a b", c=4, b=4)
                        dst = YTv[:, cb * 4:(cb + 1) * 4, :, kg * 4:(kg + 1) * 4]
                        if cb == 0:
                            T.op('act', lambda e: e.activation(out=dst, in_=src, func=AF.Copy), reads=[bk], writes=[('YT', kg, cb)])
                        else:
                            T.op('dve', lambda e: e.tensor_copy(out=dst, in_=src), reads=[bk], writes=[('YT', kg, cb)])
                T.barrier()
            if debug_stop == 'S2':
                return nc
            with contextlib.ExitStack() as esB:
                def sbb(name, shape, dtp):
                    return esB.enter_context(nc.sbuf_tensor(_PFX[0] + name, shape, dtp))
                Wo = sbb("Wo", [128, 8, 1024], BF16)
                T.op('sp', lambda e: e.dma_start(out=Wo[:], in_=woutb.rearrange("(c p) f -> p c f", p=128)), reads=['woutb'], writes=['Wo'], dsem='c0')
                xt = [sbb("Bxt%d" % i, [128, D], F32) for i in range(3)]
                ym = [sbb("Bym%d" % i, [128, D], F32) for i in range(2)]
                xo = [sbb("Bxo%d" % i, [128, D], F32) for i in range(2)]
                btiles = list(lat_tiles) + [32 + k for k in range(2 * NA)]
                NTB = len(btiles)

                def srcB(t):
                    return xown[t * 128:(t + 1) * 128, :] if t < 32 else xc[(t - 32) * 128:(t - 31) * 128, :]

                def issue_lb(n):
                    if n >= NTB:
                        return
                    i = n % 3
                    T.op('sp', lambda e: e.dma_start(out=xt[i][:], in_=srcB(btiles[n])), writes=[('Bxt', i)], dsem='Bxt%d' % i)
                issue_lb(0); issue_lb(1)
                for n in range(NTB):
                    t = btiles[n]
                    issue_lb(n + 2)
                    i = n % 3; o = n % 2
                    which = 0 if t < 32 else 1
                    for half in range(2):
                        bank = ps[(n % 2) * 2 + half]; bk = ('ps', (n % 2) * 2 + half)
                        for c in range(8):
                            lhs = YT[:, c, t * 128:(t + 1) * 128] if t < 32 else YTc[:, c, (t - 32) * 128:(t - 31) * 128]
                            T.op('pe', lambda e: e.matmul(bank[:], lhsT=lhs, rhs=Wo[:, c, half * 512:(half + 1) * 512], start=(c == 0), stop=(c == 7)),
                                 reads=['Wo'], writes=[bk])
                        T.op('dve', lambda e: e.tensor_tensor(out=ym[o][:, half * 512:(half + 1) * 512], in0=bank[:], in1=mods[(which, 2)][:, half * 512:(half + 1) * 512], op=ALU.mult),
                             reads=[bk, ('mod', which, 2)], writes=[('Bym', o, half)])
                    T.op('pool', lambda e: e.tensor_tensor(out=xo[o][:], in0=ym[o][:], in1=xt[i][:], op=ALU.add),
                         reads=[('Bym', o, 0), ('Bym', o, 1), ('Bxt', i)], writes=[('Bxo', o)])
                    T.op('sp', lambda e: e.dma_start(out=X1d[t * 128:(t + 1) * 128, :], in_=xo[o][:]), reads=[('Bxo', o)], writes=[('X1d', t)], dsem='x1w')
                T.barrier()
        if debug_stop == 'B1':
            return nc
        ffn_dense_phase(nc, T, ps, idb, mods, X1d, yown, yc, wgb, wub, wdb, lat_tiles=lat_tiles, n_ctx_tiles=2 * NA)
        T.barrier()


def ffn_dense_phase(nc, T, ps, idb, mods, X1d, yown, yc, wgb, wub, wdb, lat_tiles, n_ctx_tiles):
    with contextlib.ExitStack() as es:
        def sb(name, shape, dtp):
            return es.enter_context(nc.sbuf_tensor(_PFX[0] + name, shape, dtp))
        NC = NormCtx(nc, T, sb, idb, "F", nbuf=4)
        xt = [sb("Fxt%d" % i, [128, D], F32) for i in range(3)]
        hTs = [sb("FhT%d" % i, [128, 8, 512], BF16) for i in range(2)]
        h2T = sb("Fh2T", [128, NF, 512], BF16)
        wgs = [sb("Fwg%d" % i, [128, 8, 512], BF16) for i in range(2)]
        wus = [sb("Fwu%d" % i, [128, 8, 512], BF16) for i in range(2)]
        wds = [sb("Fwd%d" % i, [128, 1024], BF16) for i in range(4)]
        sg = [sb("Fsg%d" % i, [128, 512], F32) for i in range(2)]
        yt = [sb("Fyt%d" % i, [128, 512], F32) for i in range(2)]
        xr_ = [sb("Fxr%d" % i, [128, D], F32) for i in range(2)]
        xo = [sb("Fxo%d" % i, [128, D], F32) for i in range(2)]
        wgv = wgb.rearrange("(c p) f -> p c f", p=128)
        wuv = wub.rearrange("(c p) f -> p c f", p=128)
        n_lat_tiles = 32
        blocks = []
        lt = list(lat_tiles)
        for k in range(0, len(lt), 4):
            blocks.append((0, lt[k:k + 4]))
        if n_ctx_tiles:
            blocks.append((1, [32 + k for k in range(n_ctx_tiles)]))
        xcnt = [0]
        wcnt = [0]
        dcnt = [0]
        ecnt = [0]
        def norm_pre(bi):
            which, tiles = blocks[bi]
            slots = []
            for si, tt in enumerate(tiles):
                i = xcnt[0] % 3; xcnt[0] += 1
                T.op('sp', lambda e: e.dma_start(out=xt[i][:], in_=X1d[tt * 128:(tt + 1) * 128, :]), reads=[('X1d', tt)], writes=[('Fxt', i)], dsem='Fxt%d' % i)
                slots.append(NC.pre(xt[i][:], ('Fxt', i), mods[(which, 4)][:], ('mod', which, 4), mods[(which, 3)][:], ('mod', which, 3)))
            return slots

        def norm_post(bi, slots):
            hb_ = bi % 2
            for si, sl in enumerate(slots):
                NC.post(sl, ps[si % 2], ('ps', si % 2), hTs[hb_][:, :, si * 128:(si + 1) * 128], ('FhT', hb_, si), evac_eng='act')
        pend = norm_pre(0)
        norm_post(0, pend)
        for bi, (which, tiles) in enumerate(blocks):
            nt = len(tiles)
            ntok = nt * 128
            hT = hTs[bi % 2]
            hkeys = [('FhT', bi % 2, si) for si in range(nt)]
            for fg in range(7):
                wi = wcnt[0] % 2; wcnt[0] += 1
                T.op('sp', lambda e: e.dma_start(out=wgs[wi][:], in_=wgv[:, :, fg * 512:(fg + 1) * 512]), reads=['wgb'], writes=[('Fwg', wi)], dsem='Fwg%d' % wi)
                T.op('sp', lambda e: e.dma_start(out=wus[wi][:], in_=wuv[:, :, fg * 512:(fg + 1) * 512]), reads=['wub'], writes=[('Fwu', wi)], dsem='Fwu%d' % wi)
                for fi in range(4):
                    f = fg * 4 + fi
                    pb = (f % 2) * 2
                    for kc in range(8):
                        T.op('pe', lambda e: e.matmul(ps[pb][:, :ntok], lhsT=wgs[wi][:, kc, fi * 128:(fi + 1) * 128], rhs=hT[:, kc, :ntok], start=(kc == 0), stop=(kc == 7)),
                             reads=[('Fwg', wi)] + hkeys, writes=[('ps', pb)])
                    for kc in range(8):
                        T.op('pe', lambda e: e.matmul(ps[pb + 1][:, :ntok], lhsT=wus[wi][:, kc, fi * 128:(fi + 1) * 128], rhs=hT[:, kc, :ntok], start=(kc == 0), stop=(kc == 7)),
                             reads=[('Fwu', wi)] + hkeys, writes=[('ps', pb + 1)])
                    si_ = f % 2
                    T.op('act', lambda e: e.activation(out=sg[si_][:, :ntok], in_=ps[pb][:, :ntok], func=AF.Silu), reads=[('ps', pb)], writes=[('Fsg', si_)])
                    T.op('dve', lambda e: e.tensor_tensor(out=h2T[:, f, :ntok], in0=ps[pb + 1][:, :ntok], in1=sg[si_][:, :ntok], op=ALU.mult),
                         reads=[('ps', pb + 1), ('Fsg', si_)], writes=[('Fh2T', f)])
            nxt = norm_pre(bi + 1) if bi + 1 < len(blocks) else None
            for f in range(NF):
                di = dcnt[0] % 4; dcnt[0] += 1
                T.op('sp', lambda e: e.dma_start(out=wds[di][:], in_=wdb[f * 128:(f + 1) * 128, :]), reads=['wdb'], writes=[('Fwd', di)], dsem='Fwd%d' % di)
                for si in range(nt):
                    for half in range(2):
                        b = si * 2 + half
                        T.op('pe', lambda e: e.matmul(ps[b][:], lhsT=h2T[:, f, si * 128:(si + 1) * 128], rhs=wds[di][:, half * 512:(half + 1) * 512], start=(f == 0), stop=(f == NF - 1)),
                             reads=[('Fh2T', f), ('Fwd', di)], writes=[('ps', b)])
            for si, tt in enumerate(tiles):
                o = ecnt[0] % 2; ecnt[0] += 1
                T.op('sp', lambda e: e.dma_start(out=xr_[o][:], in_=X1d[tt * 128:(tt + 1) * 128, :]), reads=[('X1d', tt)], writes=[('Fxr', o)], dsem='Fxr%d' % o)
                for half in range(2):
                    b = si * 2 + half
                    T.op('dve', lambda e: e.tensor_tensor(out=yt[half][:], in0=ps[b][:], in1=mods[(which, 5)][:, half * 512:(half + 1) * 512], op=ALU.mult),
                         reads=[('ps', b), ('mod', which, 5)], writes=[('Fyt', half)])
                    T.op('pool', lambda e: e.tensor_tensor(out=xo[o][:, half * 512:(half + 1) * 512], in0=yt[half][:], in1=xr_[o][:, half * 512:(half + 1) * 512], op=ALU.add),
                         reads=[('Fyt', half), ('Fxr', o)], writes=[('Fxo', o, half)])
                dst = yown[tt * 128:(tt + 1) * 128, :] if tt < n_lat_tiles else yc[(tt - n_lat_tiles) * 128:(tt - n_lat_tiles + 1) * 128, :]
                T.op('sp', lambda e: e.dma_start(out=dst, in_=xo[o][:]), reads=[('Fxo', o, 0), ('Fxo', o, 1)], writes=[('yout', tt)], dsem='yout')
            if nxt is not None:
                norm_post(bi + 1, nxt)


def fnet_tables(s, shift=0):
    n = np.arange(256)
    ang = 2 * np.pi * np.outer(n, n) / 256.0
    cs256 = np.concatenate([np.cos(ang), np.sin(ang)], axis=1).astype(np.float32)
    m = np.arange(128)[:, None, None]; j = np.arange(64)[None, :, None]; kb = np.arange(128)[None, None, :]
    ph = (((j + 64 * m + shift) % 8192) * kb) % 8192
    a = 2 * np.pi * ph / 8192.0
    twc = np.cos(a).astype(np.float32).reshape(128, 64 * 128)
    tws = np.sin(a).astype(np.float32).reshape(128, 64 * 128)
    jj = np.arange(64)[:, None]; ka = (32 * s + np.arange(32))[None, :]
    a2 = 2 * np.pi * ((jj * ka) % 64) / 64.0
    w64 = np.concatenate([np.cos(a2), -np.sin(a2)], axis=0).astype(np.float32)
    return cs256, twc, tws, w64


NW = 34
NTOK = 36 * 128
CAP = 2048


def emit_attn(nc, T, ps, common, W, A, SC, s, do_ctx, final, cap=CAP, mod_load=False):
    debug_stop = None
    last = final
    idf, idb, ones = common
    xfull = A['xin']; xc = A['cin']
    yown = A['yout'][s * 4096:(s + 1) * 4096, :]; yc = A['cout']
    cvec = W['cvec']; ada_w = W['ada_w']; ada_b = W['ada_b']; nmg = W['nmg']; nfg = W['nfg']
    wq = W['wq']; wqr = W['wqr']; wk2 = W['wk2']; wk2r = W['wk2r']; wv = W['wv']; wo = W['wo']
    cost = W['cost_%d' % s]; sint = W['sint_%d' % s]; sinks = W['sinks']; masks = W['masks_%d' % s]; wr = W['wr']
    mwg = W['mwg']; mwu = W['mwu']; mwd = W['mwd']; fng = W['fng']; ut_d = W['ut']; ebase_d = W['ebase']
    hTd = SC['hTd']; qTd = SC['qTd']; X1d = SC['X1d']; hbuf = SC['hbuf']; ybuf = SC['ybuf']
    n_ctx_q = 2 if do_ctx else 0
    n_moe_tiles = 32 + n_ctx_q

    def win_tile(tt):
        r0 = (s * 4096 - 128 + tt * 128) % S
        return xfull[r0:r0 + 128, :]

    with contextlib.ExitStack() as es:
        def sbg(name, shape, dtp):
            return es.enter_context(nc.sbuf_tensor(_PFX[0] + name, shape, dtp))
        mods = {}
        for which in range(2):
            for idx in (3, 4, 5):
                mods[(which, idx)] = sbg("mod%d_%d" % (which, idx), [128, D], F32)
        with contextlib.ExitStack() as es_mix:
            def sbm(name, shape, dtp):
                return es_mix.enter_context(nc.sbuf_tensor(_PFX[0] + name, shape, dtp))
            for which in range(2):
                for idx in (0, 1, 2):
                    mods[(which, idx)] = sbm("mod%d_%d" % (which, idx), [128, D], F32)
            compute_mod(nc, T, None, ps, cvec, ada_w, ada_b, ones, mods,
                        [(nmg, [(0, 1), (1, 1)]), (nfg, [(0, 4), (1, 4)])], cache=SC['modc'], load=mod_load)
            kT2 = sbm("kT2", [128, 4, NTOK], BF16)
            V = sbm("V", [128, 36, 256], BF16)
            with contextlib.ExitStack() as esK:
                def sbk(name, shape, dtp):
                    return esK.enter_context(nc.sbuf_tensor(_PFX[0] + name, shape, dtp))
                NC = NormCtx(nc, T, sbk, idb, "K")
                Wk2 = sbk("Wk2", [128, 8, 512], BF16); Wk2r = sbk("Wk2r", [128, 8, 512], BF16); Wv = sbk("Wv", [128, 8, 256], BF16)
                T.op('pool', lambda e: e.dma_start(out=Wk2[:], in_=wk2.rearrange("(c p) f -> p c f", p=128)), writes=['Wk2'], dsem='c1')
                T.op('pool', lambda e: e.dma_start(out=Wk2r[:], in_=wk2r.rearrange("(c p) f -> p c f", p=128)), writes=['Wk2r'], dsem='c1')
                T.op('pool', lambda e: e.dma_start(out=Wv[:], in_=wv.rearrange("(c p) f -> p c f", p=128)), writes=['Wv'], dsem='c1')
                xt = [sbk("Kxt%d" % i, [128, D], F32) for i in range(3)]
                hT = [sbk("KhT%d" % i, [128, 8, 512], BF16) for i in range(2)]
                ct = [sbk("Kct%d" % i, [128, 512], F32) for i in range(2)]
                st = [sbk("Kst%d" % i, [128, 512], F32) for i in range(2)]
                t1 = [sbk("Kt1_%d" % i, [128, 512], F32) for i in range(2)]
                t2 = [sbk("Kt2_%d" % i, [128, 512], F32) for i in range(2)]
                xcnt = 0
                for g in range(9):
                    hi = g % 2
                    T.op('sp', lambda e: e.dma_start(out=ct[hi][:], in_=cost[:, g * 512:(g + 1) * 512]), writes=[('Kct', hi)], dsem='Kct%d' % hi)
                    T.op('sp', lambda e: e.dma_start(out=st[hi][:], in_=sint[:, g * 512:(g + 1) * 512]), writes=[('Kst', hi)], dsem='Kst%d' % hi)
                    for si in range(4):
                        tt = g * 4 + si
                        which = 0 if tt < NW else 1
                        src = win_tile(tt) if tt < NW else xc[(tt - NW) * 128:(tt - NW + 1) * 128, :]
                        i = xcnt % 3; xcnt += 1
                        T.op('sp', lambda e: e.dma_start(out=xt[i][:], in_=src), writes=[('Kxt', i)], dsem='Kxt%d' % i)
                        NC.run(xt[i][:], ('Kxt', i), mods[(which, 1)][:], ('mod', which, 1), mods[(which, 0)][:], ('mod', which, 0),
                               ps[6 + si % 2], ('ps', 6 + si % 2), hT[hi][:, :, si * 128:(si + 1) * 128], ('KhT', hi, si), evac_eng='act')
                        bank = ps[4 + si % 2]; bk = ('ps', 4 + si % 2)
                        for kc in range(8):
                            T.op('pe', lambda e: e.matmul(bank[:, 0:256], lhsT=hT[hi][:, kc, si * 128:(si + 1) * 128], rhs=Wv[:, kc, :], start=(kc == 0), stop=(kc == 7)),
                                 reads=[('KhT', hi, si), 'Wv'], writes=[bk])
                        T.op('act', lambda e: e.activation(out=V[:, tt, :], in_=bank[:, 0:256], func=AF.Copy), reads=[bk], writes=[('V', tt)])
                    hkeys = [('KhT', hi, si) for si in range(4)]
                    T.op('sp', lambda e: e.dma_start(out=hTd[:, :, g * 512:(g + 1) * 512], in_=hT[hi][:]), reads=hkeys, writes=[('hTd', g)], dsem='htw')
                    for kv in range(4):
                        b0 = ps[(kv % 2) * 2]; k0 = ('ps', (kv % 2) * 2)
                        b1 = ps[(kv % 2) * 2 + 1]; k1 = ('ps', (kv % 2) * 2 + 1)
                        for kc in range(8):
                            T.op('pe', lambda e: e.matmul(b0[:], lhsT=Wk2[:, kc, kv * 128:(kv + 1) * 128], rhs=hT[hi][:, kc, :], start=(kc == 0), stop=(kc == 7)),
                                 reads=hkeys + ['Wk2'], writes=[k0])
                        for kc in range(8):
                            T.op('pe', lambda e: e.matmul(b1[:], lhsT=Wk2r[:, kc, kv * 128:(kv + 1) * 128], rhs=hT[hi][:, kc, :], start=(kc == 0), stop=(kc == 7)),
                                 reads=hkeys + ['Wk2r'], writes=[k1])
                        ti = kv % 2
                        T.op('dve', lambda e: e.tensor_tensor(out=t1[ti][:], in0=b0[:], in1=ct[hi][:], op=ALU.mult), reads=[k0, ('Kct', hi)], writes=[('Kt1', ti)])
                        T.op('dve', lambda e: e.tensor_tensor(out=t2[ti][:], in0=b1[:], in1=st[hi][:], op=ALU.mult), reads=[k1, ('Kst', hi)], writes=[('Kt2', ti)])
                        T.op('pool', lambda e: e.tensor_tensor(out=kT2[:, kv, g * 512:(g + 1) * 512], in0=t1[ti][:], in1=t2[ti][:], op=ALU.add),
                             reads=[('Kt1', ti), ('Kt2', ti)], writes=[('kT2', kv, g)])
                T.barrier()
            if debug_stop == 'K':
                return nc
            with contextlib.ExitStack() as esQ:
                def sbq(name, shape, dtp):
                    return esQ.enter_context(nc.sbuf_tensor(_PFX[0] + name, shape, dtp))
                Wq = sbq("Wq", [128, 8, 1024], BF16); Wqr = sbq("Wqr", [128, 8, 1024], BF16)
                T.op('pool', lambda e: e.dma_start(out=Wq[:], in_=wq.rearrange("(c p) f -> p c f", p=128)), writes=['Wq'], dsem='c1')
                T.op('pool', lambda e: e.dma_start(out=Wqr[:], in_=wqr.rearrange("(c p) f -> p c f", p=128)), writes=['Wqr'], dsem='c1')
                hT = [sbq("QhT%d" % i, [128, 8, 512], BF16) for i in range(2)]
                qo = [sbq("Qqo%d" % i, [128, 8, 512], BF16) for i in range(2)]
                ct = [sbq("Qct%d" % i, [128, 512], F32) for i in range(2)]
                st = [sbq("Qst%d" % i, [128, 512], F32) for i in range(2)]
                t1 = [sbq("Qt1_%d" % i, [128, 512], F32) for i in range(2)]
                t2 = [sbq("Qt2_%d" % i, [128, 512], F32) for i in range(2)]
                groups = [(128 + 512 * g, 512, 512 * g) for g in range(8)]
                if n_ctx_q:
                    groups.append((NW * 128, 256, 4096))
                for gi, (tok0, n, qoff) in enumerate(groups):
                    hi = gi % 2
                    T.op('sp', lambda e: e.dma_start(out=hT[hi][:, :, :n], in_=hTd[:, :, tok0:tok0 + n]), reads=[('hTd', g) for g in range(9)], writes=[('QhT', hi)], dsem='QhT%d' % hi)
                    T.op('sp', lambda e: e.dma_start(out=ct[hi][:, :n], in_=cost[:, tok0:tok0 + n]), writes=[('Qct', hi)], dsem='Qct%d' % hi)
                    T.op('sp', lambda e: e.dma_start(out=st[hi][:, :n], in_=sint[:, tok0:tok0 + n]), writes=[('Qst', hi)], dsem='Qst%d' % hi)
                    for c in range(8):
                        b0 = ps[(c % 4) * 2]; k0 = ('ps', (c % 4) * 2)
                        b1 = ps[(c % 4) * 2 + 1]; k1 = ('ps', (c % 4) * 2 + 1)
                        for kc in range(8):
                            T.op('pe', lambda e: e.matmul(b0[:, :n], lhsT=Wq[:, kc, c * 128:(c + 1) * 128], rhs=hT[hi][:, kc, :n], start=(kc == 0), stop=(kc == 7)),
                                 reads=[('QhT', hi), 'Wq'], writes=[k0])
                        for kc in range(8):
                            T.op('pe', lambda e: e.matmul(b1[:, :n], lhsT=Wqr[:, kc, c * 128:(c + 1) * 128], rhs=hT[hi][:, kc, :n], start=(kc == 0), stop=(kc == 7)),
                                 reads=[('QhT', hi), 'Wqr'], writes=[k1])
                        ti = c % 2
                        T.op('dve', lambda e: e.tensor_tensor(out=t1[ti][:, :n], in0=b0[:, :n], in1=ct[hi][:, :n], op=ALU.mult), reads=[k0, ('Qct', hi)], writes=[('Qt1', ti)])
                        T.op('dve', lambda e: e.tensor_tensor(out=t2[ti][:, :n], in0=b1[:, :n], in1=st[hi][:, :n], op=ALU.mult), reads=[k1, ('Qst', hi)], writes=[('Qt2', ti)])
                        T.op('pool', lambda e: e.tensor_tensor(out=qo[hi][:, c, :n], in0=t1[ti][:, :n], in1=t2[ti][:, :n], op=ALU.add),
                             reads=[('Qt1', ti), ('Qt2', ti)], writes=[('Qqo', hi, c)])
                    T.op('sp', lambda e: e.dma_start(out=qTd[:, :, qoff:qoff + n], in_=qo[hi][:, :, :n]), reads=[('Qqo', hi, c) for c in range(8)], writes=[('qTd', gi)], dsem='qtw')
                T.barrier()
            if debug_stop == 'Q':
                return nc
            with contextlib.ExitStack() as esA:
                def sba(name, shape, dtp):
                    return esA.enter_context(nc.sbuf_tensor(_PFX[0] + name, shape, dtp))
                Wo = sba("Wo", [64, 16, 1024], BF16)
                T.op('pool', lambda e: e.dma_start(out=Wo[:], in_=wo.rearrange("(h d) f -> d h f", d=64)), writes=['Wo'], dsem='c1')
                mk = sba("mk", [128, 512], BF16)
                T.op('pool', lambda e: e.dma_start(out=mk[:], in_=masks), writes=['mk'], dsem='c1')
                sk = sba("sk", [64, 16], F32); esk = sba("esk", [64, 16], F32)
                T.op('sp', lambda e: e.dma_start(out=sk[:], in_=sinks.partition_broadcast(64).squeeze(1)), writes=['sk'], dsem='c0')
                T.op('act', lambda e: e.activation(out=esk[:], in_=sk[:], func=AF.Exp), reads=['sk'], writes=['esk'])
                ES = sba("ES", [64, 4, 512], F32)
                for kv in range(4):
                    for slot in range(4):
                        half, cc = slot // 2, slot % 2
                        hd = 4 * kv + 2 * cc + half
                        T.op('dve', lambda e: e.tensor_scalar(out=ES[:, kv, slot * 128:(slot + 1) * 128], in0=ones[0:64, :], scalar1=esk[:, hd:hd + 1], scalar2=None, op0=ALU.mult),
                             reads=['ones', 'esk'], writes=[('ES', kv, slot)])
                ones_b = sba("ones_b", [128, 64], BF16)
                T.op('pool', lambda e: e.memset(ones_b[:], 1.0), writes=['ones_b'])
                qt = [sba("Aqt%d" % i, [128, 8, 512], BF16) for i in range(2)]
                PT = [sba("APT%d" % i, [128, 512], BF16) for i in range(10)]
                oT = [sba("AoT%d" % i, [64, 4, 512], BF16) for i in range(2)]
                dn = [sba("Adn%d" % i, [64, 512], F32) for i in range(2)]
                xt = [sba("Axt%d" % i, [128, D], F32) for i in range(2)]
                ym = [sba("Aym%d" % i, [128, D], F32) for i in range(2)]
                xo = [sba("Axo%d" % i, [128, D], F32) for i in range(2)]
                ptc = 0
                nqb = 32 + n_ctx_q
                qkeys = [('qTd', g) for g in range(9)]
                state = {'ptc': 0}

                def qb_info(qb):
                    is_ctx = qb >= 32
                    w = qb + 1
                    if is_ctx:
                        chunks = [(34, None), (35, None)]
                    else:
                        mP = 1 if qb == 0 else 0
                        mN = 3 if qb == 31 else 2
                        chunks = [(w - 1, mP), (w, None), (w + 1, mN), (34, None), (35, None)]
                    return is_ctx, w, chunks

                def emit_scores(qb, kv):
                    qi = (qb // 4) % 2
                    if qb % 4 == 0 and kv == 0:
                        n = 512 if qb < 32 else 256
                        T.op('sp', lambda e: e.dma_start(out=qt[qi][:, :, :n], in_=qTd[:, :, qb * 128: qb * 128 + n]), reads=qkeys, writes=[('Aqt', qi)], dsem='Aqt%d' % qi)
                    qo_ = (qb % 4) * 128
                    is_ctx, w, chunks = qb_info(qb)
                    pts = []
                    for (kt, mi) in chunks:
                        ptc = state['ptc']
                        sb_i = (ptc % 2) * 2
                        pi = ptc % 10; state['ptc'] = ptc + 1
                        for half in range(2):
                            bank = ps[sb_i + half]; bk = ('ps', sb_i + half)
                            T.op('pe', lambda e: e.matmul(bank[:, 0:256].rearrange("p (c q) -> p c q", c=2),
                                                          lhsT=kT2[half * 64:(half + 1) * 64, kv, kt * 128:(kt + 1) * 128],
                                                          rhs=qt[qi][half * 64:(half + 1) * 64, 2 * kv:2 * kv + 2, qo_:qo_ + 128], start=True, stop=True),
                                 reads=[('Aqt', qi)], writes=[bk])
                            T.op('act', lambda e: e.activation(out=PT[pi][:, half * 256:(half + 1) * 256], in_=bank[:, 0:256], func=AF.Exp, scale=0.125), reads=[bk], writes=[('APT', pi, half)])
                        if mi is not None:
                            T.op('pool', lambda e: e.tensor_tensor(out=PT[pi][:].rearrange("p (s q) -> p s q", s=4), in0=PT[pi][:].rearrange("p (s q) -> p s q", s=4),
                                                                  in1=mk[:, mi * 128:(mi + 1) * 128].unsqueeze(1).to_broadcast([128, 4, 128]), op=ALU.mult),
                                 reads=[('APT', pi, 0), ('APT', pi, 1), 'mk'], writes=[('APT', pi, 0), ('APT', pi, 1)])
                        pts.append((pi, kt))
                    return pts

                def emit_pv(qb, kv, pts):
                    oi = qb % 2
                    ob = ps[4 + kv % 2]; obk = ('ps', 4 + kv % 2)
                    db = ps[6 + kv % 2]; dbk = ('ps', 6 + kv % 2)
                    for ci, (pi, kt) in enumerate(pts):
                        T.op('pe', lambda e: e.matmul(ob[0:64, :], lhsT=V[:, kt, kv * 64:(kv + 1) * 64], rhs=PT[pi][:], start=(ci == 0), stop=(ci == len(pts) - 1)),
                             reads=[('APT', pi, 0), ('APT', pi, 1)], writes=[obk])
                    for ci, (pi, kt) in enumerate(pts):
                        T.op('pe', lambda e: e.matmul(db[0:64, :], lhsT=ones_b[:, :], rhs=PT[pi][:], start=(ci == 0), stop=(ci == len(pts) - 1)),
                             reads=[('APT', pi, 0), ('APT', pi, 1), 'ones_b'], writes=[dbk])
                    di = kv % 2
                    T.op('dve', lambda e: e.tensor_tensor(out=dn[di][:], in0=db[0:64, :], in1=ES[:, kv, :], op=ALU.add), reads=[dbk, ('ES', kv, 0), ('ES', kv, 1), ('ES', kv, 2), ('ES', kv, 3)], writes=[('Adn', di)])
                    T.op('dve', lambda e: e.reciprocal(out=dn[di][:], in_=dn[di][:]), reads=[('Adn', di)], writes=[('Adn', di)])
                    T.op('dve', lambda e: e.tensor_tensor(out=oT[oi][:, kv, :], in0=ob[0:64, :], in1=dn[di][:], op=ALU.mult), reads=[obk, ('Adn', di)], writes=[('AoT', oi, kv)])

                steps = [(qb, kv) for qb in range(nqb) for kv in range(4)]
                nxt_pts = emit_scores(*steps[0])
                for sn, (qb, kv) in enumerate(steps):
                    cur_pts = nxt_pts
                    if sn + 1 < len(steps):
                        nxt_pts = emit_scores(*steps[sn + 1])
                    emit_pv(qb, kv, cur_pts)
                    if kv != 3:
                        continue
                    is_ctx, w, chunks = qb_info(qb)
                    oi = qb % 2
                    xi = qb % 2
                    src = win_tile(w) if not is_ctx else xc[(qb - 32) * 128:(qb - 31) * 128, :]
                    which = 1 if is_ctx else 0
                    T.op('sp', lambda e: e.dma_start(out=xt[xi][:], in_=src), writes=[('Axt', xi)], dsem='Axt%d' % xi)
                    for hf in range(2):
                        yb = ps[4 + hf] if False else ps[(qb % 2) * 2 + hf]
                        ybk = ('ps', (qb % 2) * 2 + hf)
                        n_acc = 0
                        for kv in range(4):
                            for slot in range(4):
                                half, cc = slot // 2, slot % 2
                                hd = 4 * kv + 2 * cc + half
                                T.op('pe', lambda e: e.matmul(yb[:], lhsT=oT[oi][:, kv, slot * 128:(slot + 1) * 128], rhs=Wo[:, hd, hf * 512:(hf + 1) * 512], start=(n_acc == 0), stop=(n_acc == 15)),
                                     reads=[('AoT', oi, kv), 'Wo'], writes=[ybk])
                                n_acc += 1
                        T.op('dve', lambda e: e.tensor_tensor(out=ym[xi][:, hf * 512:(hf + 1) * 512], in0=yb[:], in1=mods[(which, 2)][:, hf * 512:(hf + 1) * 512], op=ALU.mult),
                             reads=[ybk, ('mod', which, 2)], writes=[('Aym', xi, hf)])
                    T.op('pool', lambda e: e.tensor_tensor(out=xo[xi][:], in0=ym[xi][:], in1=xt[xi][:], op=ALU.add), reads=[('Aym', xi, 0), ('Aym', xi, 1), ('Axt', xi)], writes=[('Axo', xi)])
                    T.op('sp', lambda e: e.dma_start(out=X1d[qb * 128:(qb + 1) * 128, :], in_=xo[xi][:]), reads=[('Axo', xi)], writes=[('X1d', qb)], dsem='x1w')
                T.barrier()
        if debug_stop == 'ATT':
            with contextlib.ExitStack() as esD:
                tmp = esD.enter_context(nc.sbuf_tensor(_PFX[0] + "dbg", [128, D], F32))
                for t in range(34):
                    T.op('sp', lambda e: e.dma_start(out=tmp[:], in_=X1d[t * 128:(t + 1) * 128, :]), writes=['dbg'], dsem='dbg')
                    dst = yown[t * 128:(t + 1) * 128, :] if t < 32 else yc[(t - 32) * 128:(t - 31) * 128, :]
                    T.op('sp', lambda e: e.dma_start(out=dst, in_=tmp[:]), reads=['dbg'], writes=[('o', t)], dsem='dbg2')
                T.barrier()
            return nc
        moe_phase(nc, T, ps, idb, idf, ones, mods, X1d, yown, yc, wr, mwg, mwu, mwd, hbuf, ybuf, fng, n_moe_tiles, last, cap, ut_d, ebase_d)
        T.barrier()


def moe_phase(nc, T, ps, idb, idf, ones, mods, X1d, yown, yc, wr, mwg, mwu, mwd, hbuf, ybuf, fng, ntiles, last, cap, ut_d, ebase_d):
    with contextlib.ExitStack() as es0:
        def sb0(name, shape, dtp):
            return es0.enter_context(nc.sbuf_tensor(_PFX[0] + name, shape, dtp))
        bc_reg = nc.gpsimd.to_reg(8 * cap - 1)
        dest = sb0("dest", [128, ntiles * 2], I32)
        gate = sb0("gate", [128, ntiles * 2], F32)
        with contextlib.ExitStack() as es:
            def sb(name, shape, dtp):
                return es.enter_context(nc.sbuf_tensor(_PFX[0] + name, shape, dtp))
            NC = NormCtx(nc, T, sb, idb, "M", nbuf=4)
            Wr = sb("Wr", [128, 8, 8], F32)
            T.op('sp', lambda e: e.dma_start(out=Wr[:], in_=wr.rearrange("(c p) f -> p c f", p=128)), writes=['Wr'], dsem='c0')
            UT = sb("UT", [128, 128], F32)
            T.op('sp', lambda e: e.dma_start(out=UT[:], in_=ut_d), writes=['UT'], dsem='c0')
            ebase = sb("ebase_sb", [128, 8], F32)
            T.op('sp', lambda e: e.dma_start(out=ebase[:], in_=ebase_d), writes=[('ebase', ex) for ex in range(8)], dsem='c0')
            cnt = sb("cnt", [128, 8], F32)
            T.op('pool', lambda e: e.memset(cnt[:], 0.0), writes=['cnt'])
            xt = [sb("Mxt%d" % i, [128, D], F32) for i in range(4)]
            hTf = [sb("MhTf%d" % i, [128, 8, 128], F32) for i in range(4)]
            lg = [sb("Mlg%d" % i, [128, 8], F32) for i in range(4)]
            m8 = [sb("Mm8%d" % i, [128, 8], F32) for i in range(4)]
            sel = [sb("Msel%d" % i, [128, 8], F32) for i in range(4)]
            mk1 = [sb("Mmk1%d" % i, [128, 8], F32) for i in range(4)]
            mk2 = [sb("Mmk2%d" % i, [128, 8], F32) for i in range(4)]
            rk = [sb("Mrk%d" % i, [128, 8], F32) for i in range(4)]
            tmp8 = [sb("Mt8%d" % i, [128, 8], F32) for i in range(4)]
            df = [sb("Mdf%d" % i, [128, 2], F32) for i in range(4)]
            ex_ = [sb("Mex%d" % i, [128, 2], F32) for i in range(4)]
            ekeys = [('ebase', ex) for ex in range(8)]
            for t in range(ntiles):
                which = 0 if t < 32 else 1
                i = t % 4; o = t % 4
                T.op('sp', lambda e: e.dma_start(out=xt[i][:], in_=X1d[t * 128:(t + 1) * 128, :]), reads=[('X1d', t)], writes=[('Mxt', i)], dsem='Mxt%d' % i)
                r = NC.r.next()
                ss, h1, hb = NC.ss[r], NC.h1[r], NC.hb[r]
                T.op('act', lambda e: e.activation(out=NC.junk[:], in_=xt[i][:], func=AF.Square, accum_out=ss[:, 0:1]), reads=[('Mxt', i)], writes=[('Mss', r)])
                T.op('dve', lambda e: e.tensor_scalar(out=ss[:, 1:2], in0=ss[:, 0:1], scalar1=1.0 / D, scalar2=EPS, op0=ALU.mult, op1=ALU.add), reads=[('Mss', r)], writes=[('Mss1', r)])
                T.op('pool', lambda e: e.tensor_tensor(out=ss[:, 1:2], in0=ss[:, 1:2], in1=NC.nhalf[:], op=ALU.pow), reads=[('Mss1', r), 'Mnhalf'], writes=[('Mss1', r)])
                T.op('dve', lambda e: e.scalar_tensor_tensor(out=h1[:], in0=xt[i][:], scalar=ss[:, 1:2], in1=mods[(which, 4)][:], op0=ALU.mult, op1=ALU.mult),
                     reads=[('Mxt', i), ('Mss1', r), ('mod', which, 4)], writes=[('Mh1', r)])
                T.op('pool', lambda e: e.tensor_tensor(out=h1[:], in0=h1[:], in1=mods[(which, 3)][:], op=ALU.add), reads=[('Mh1', r), ('mod', which, 3)], writes=[('Mh1', r)])
                T.op('act', lambda e: e.activation(out=hb[:], in_=h1[:], func=AF.Copy), reads=[('Mh1', r)], writes=[('Mhb', r)])
                for half in range(2):
                    bank = ps[(t % 2) * 2 + half]; bk = ('ps', (t % 2) * 2 + half)
                    for c in range(4):
                        cc = half * 4 + c
                        T.op('pe', lambda e: e.transpose(out=bank[:, c * 128:(c + 1) * 128], in_=h1[:, cc * 128:(cc + 1) * 128], identity=idf[:]), reads=[('Mh1', r), 'idf'], writes=[bk])
                    T.op('dve' if half == 0 else 'act',
                         (lambda e: e.tensor_copy(out=hTf[o][:, 0:4, :], in_=bank[:].rearrange("p (c t) -> p c t", c=4))) if half == 0 else
                         (lambda e: e.activation(out=hTf[o][:, 4:8, :], in_=bank[:].rearrange("p (c t) -> p c t", c=4), func=AF.Copy)),
                         reads=[bk], writes=[('MhTf', o, half)])
                lb = ps[4 + t % 2]; lbk = ('ps', 4 + t % 2)
                for kc in range(8):
                    T.op('pe', lambda e: e.matmul(lb[:, 0:8], lhsT=hTf[o][:, kc, :], rhs=Wr[:, kc, :], start=(kc == 0), stop=(kc == 7)), reads=[('MhTf', o, kc // 4), 'Wr'], writes=[lbk])
                T.op('dve', lambda e: e.tensor_copy(out=lg[o][:], in_=lb[:, 0:8]), reads=[lbk], writes=[('Mlg', o)])
                T.op('dve', lambda e: e.max(out=m8[o][:], in_=lg[o][:]), reads=[('Mlg', o)], writes=[('Mm8', o)])
                T.op('dve', lambda e: e.tensor_scalar(out=mk1[o][:], in0=lg[o][:], scalar1=m8[o][:, 0:1], scalar2=None, op0=ALU.is_equal), reads=[('Mlg', o), ('Mm8', o)], writes=[('Mmk1', o)])
                T.op('dve', lambda e: e.tensor_scalar(out=mk2[o][:], in0=lg[o][:], scalar1=m8[o][:, 1:2], scalar2=None, op0=ALU.is_equal), reads=[('Mlg', o), ('Mm8', o)], writes=[('Mmk2', o)])
                T.op('dve', lambda e: e.tensor_tensor(out=sel[o][:], in0=mk1[o][:], in1=mk2[o][:], op=ALU.add), reads=[('Mmk1', o), ('Mmk2', o)], writes=[('Msel', o)])
                T.op('dve', lambda e: e.tensor_tensor(out=df[o][:, 0:1], in0=m8[o][:, 1:2], in1=m8[o][:, 0:1], op=ALU.subtract), reads=[('Mm8', o)], writes=[('Mdf', o)])
                T.op('act', lambda e: e.activation(out=ex_[o][:, 0:1], in_=df[o][:, 0:1], func=AF.Exp), reads=[('Mdf', o)], writes=[('Mex', o)])
                T.op('dve', lambda e: e.tensor_scalar(out=ex_[o][:, 1:2], in0=ex_[o][:, 0:1], scalar1=1.0, scalar2=None, op0=ALU.add), reads=[('Mex', o)], writes=[('Mex1', o)])
                T.op('dve', lambda e: e.reciprocal(out=gate[:, 2 * t:2 * t + 1], in_=ex_[o][:, 1:2]), reads=[('Mex1', o)], writes=[('gate', t, 0)])
                T.op('dve', lambda e: e.tensor_scalar(out=gate[:, 2 * t + 1:2 * t + 2], in0=gate[:, 2 * t:2 * t + 1], scalar1=-1.0, scalar2=1.0, op0=ALU.mult, op1=ALU.add), reads=[('gate', t, 0)], writes=[('gate', t, 1)])
                rb = ps[6 + t % 2]; rbk = ('ps', 6 + t % 2)
                T.op('pe', lambda e: e.matmul(rb[:, 0:8], lhsT=UT[:], rhs=sel[o][:], start=True, stop=True), reads=['UT', ('Msel', o)], writes=[rbk])
                T.op('pe', lambda e: e.matmul(rb[:, 8:16], lhsT=ones[:], rhs=sel[o][:], start=True, stop=True), reads=['ones', ('Msel', o)], writes=[rbk])
                T.op('dve', lambda e: e.tensor_tensor(out=rk[o][:], in0=rb[:, 0:8], in1=cnt[:], op=ALU.add), reads=[rbk, 'cnt'], writes=[('Mrk', o)])
                T.op('dve', lambda e: e.tensor_tensor(out=cnt[:], in0=rb[:, 8:16], in1=cnt[:], op=ALU.add), reads=[rbk, 'cnt', ('Mrk', o)], writes=['cnt'])
                T.op('dve', lambda e: e.tensor_scalar(out=tmp8[o][:], in0=rk[o][:], scalar1=float(cap), scalar2=1.0e7, op0=ALU.is_ge, op1=ALU.mult), reads=[('Mrk', o)], writes=[('Mt8', o)])
                T.op('dve', lambda e: e.tensor_tensor(out=rk[o][:], in0=rk[o][:], in1=tmp8[o][:], op=ALU.add), reads=[('Mrk', o), ('Mt8', o)], writes=[('Mrk', o)])
                T.op('dve', lambda e: e.tensor_tensor(out=rk[o][:], in0=rk[o][:], in1=ebase[:], op=ALU.add), reads=[('Mrk', o)] + ekeys, writes=[('Mrk', o)])
                for k, mk_ in enumerate((mk1, mk2)):
                    T.op('dve', lambda e: e.tensor_tensor(out=tmp8[o][:], in0=rk[o][:], in1=mk_[o][:], op=ALU.mult), reads=[('Mrk', o), ('Mmk%d' % (k + 1), o)], writes=[('Mt8', o)])
                    T.op('dve', lambda e: e.tensor_reduce(out=df[o][:, 1:2], in_=tmp8[o][:], axis=AX.X, op=ALU.add), reads=[('Mt8', o)], writes=[('Mdf1', o)])
                    T.op('dve', lambda e: e.tensor_copy(out=dest[:, 2 * t + k:2 * t + k + 1], in_=df[o][:, 1:2]), reads=[('Mdf1', o)], writes=[('dest', t, k)])
                    T.op('pool', lambda e: e.indirect_dma_start(out=hbuf[:, :], out_offset=bass.IndirectOffsetOnAxis(ap=dest[:, 2 * t + k:2 * t + k + 1], axis=0), in_=hb[:, :], in_offset=None,
                                                                bounds_check=bc_reg, oob_is_err=False),
                         reads=[('dest', t, k), ('Mhb', r)], writes=[('hbuf', t, k)], dsem='hsc')
            T.barrier()
        nst = cap // 512
        with contextlib.ExitStack() as es:
            def sb(name, shape, dtp):
                return es.enter_context(nc.sbuf_tensor(_PFX[0] + name, shape, dtp))
            hTe = sb("EhT", [128, 8, cap], BF16)
            acc = sb("Eacc", [128, cap // 128, D], F32)
            hg = [sb("Ehg%d" % i, [128, D], BF16) for i in range(3)]
            wgs = [sb("Ewg%d" % i, [128, 8, 512], BF16) for i in range(2)]
            wus = [sb("Ewu%d" % i, [128, 8, 512], BF16) for i in range(2)]
            wds = [sb("Ewd%d" % i, [128, 4, D], BF16) for i in range(2)]
            h2 = [sb("Eh2_%d" % i, [128, 4, 512], BF16) for i in range(2)]
            sg = [sb("Esg%d" % i, [128, 512], F32) for i in range(2)]
            wcnt = 0; gcnt = 0; hcnt = 0; bcnt = 0
            for ex in range(8):
                for s_ in range(cap // 128):
                    gi = gcnt % 3; gcnt += 1
                    T.op('sp', lambda e: e.dma_start(out=hg[gi][:], in_=hbuf[ex * cap + s_ * 128: ex * cap + (s_ + 1) * 128, :]), reads=['hbuf_all'], writes=[('Ehg', gi)], dsem='Ehg%d' % gi)
                    bank = ps[6 + s_ % 2]; bk = ('ps', 6 + s_ % 2)
                    pT = bank[:].bitcast(BF16).rearrange("p (c t) -> p c t", c=8)
                    for c in range(8):
                        T.op('pe', lambda e: e.transpose(out=pT[:, c, :], in_=hg[gi][:, c * 128:(c + 1) * 128], identity=idb[:]), reads=[('Ehg', gi), 'idb'], writes=[bk])
                    if s_ % 2 == 0:
                        T.op('act', lambda e: e.activation(out=hTe[:, :, s_ * 128:(s_ + 1) * 128], in_=pT, func=AF.Copy), reads=[bk], writes=[('EhT', s_)])
                    else:
                        T.op('dve', lambda e: e.tensor_copy(out=hTe[:, :, s_ * 128:(s_ + 1) * 128], in_=pT), reads=[bk], writes=[('EhT', s_)])
                for fg in range(7):
                    wi = wcnt % 2; wcnt += 1
                    T.op('pool', lambda e: e.dma_start(out=wgs[wi][:], in_=mwg[ex].rearrange("(c p) f -> p c f", p=128)[:, :, fg * 512:(fg + 1) * 512]), writes=[('Ewg', wi)], dsem='Ewg%d' % wi)
                    T.op('pool', lambda e: e.dma_start(out=wus[wi][:], in_=mwu[ex].rearrange("(c p) f -> p c f", p=128)[:, :, fg * 512:(fg + 1) * 512]), writes=[('Ewu', wi)], dsem='Ewu%d' % wi)
                    T.op('pool', lambda e: e.dma_start(out=wds[wi][:], in_=mwd[ex][fg * 512:(fg + 1) * 512, :].rearrange("(c p) f -> p c f", p=128)), writes=[('Ewd', wi)], dsem='Ewd%d' % wi)
                    for st_ in range(nst):
                        hi = hcnt % 2; hcnt += 1
                        hk = [('EhT', st_ * 4 + q) for q in range(4)]
                        for fi in range(4):
                            pb = (bcnt % 2) * 2; bcnt += 1
                            for kc in range(8):
                                T.op('pe', lambda e: e.matmul(ps[pb][:], lhsT=wgs[wi][:, kc, fi * 128:(fi + 1) * 128], rhs=hTe[:, kc, st_ * 512:(st_ + 1) * 512], start=(kc == 0), stop=(kc == 7)),
                                     reads=[('Ewg', wi)] + hk, writes=[('ps', pb)])
                            for kc in range(8):
                                T.op('pe', lambda e: e.matmul(ps[pb + 1][:], lhsT=wus[wi][:, kc, fi * 128:(fi + 1) * 128], rhs=hTe[:, kc, st_ * 512:(st_ + 1) * 512], start=(kc == 0), stop=(kc == 7)),
                                     reads=[('Ewu', wi)] + hk, writes=[('ps', pb + 1)])
                            si_ = fi % 2
                            T.op('act', lambda e: e.activation(out=sg[si_][:], in_=ps[pb][:], func=AF.Silu), reads=[('ps', pb)], writes=[('Esg', si_)])
                            T.op('dve', lambda e: e.tensor_tensor(out=h2[hi][:, fi, :], in0=ps[pb + 1][:], in1=sg[si_][:], op=ALU.mult), reads=[('ps', pb + 1), ('Esg', si_)], writes=[('Eh2', hi, fi)])
                        for sub in range(4):
                            for half in range(2):
                                ab = 4 + ((sub * 2 + half) % 4); abk = ('ps', ab)
                                for fi in range(4):
                                    T.op('pe', lambda e: e.matmul(ps[ab][:], lhsT=h2[hi][:, fi, sub * 128:(sub + 1) * 128], rhs=wds[wi][:, fi, half * 512:(half + 1) * 512], start=(fi == 0), stop=(fi == 3)),
                                         reads=[('Eh2', hi, fi), ('Ewd', wi)], writes=[abk])
                                sl = st_ * 4 + sub
                                dsta = acc[:, sl, half * 512:(half + 1) * 512]
                                if fg == 0:
                                    T.op('act', lambda e: e.activation(out=dsta, in_=ps[ab][:], func=AF.Copy), reads=[abk], writes=[('Eacc', sl, half)])
                                else:
                                    T.op('dve', lambda e: e.tensor_tensor(out=dsta, in0=ps[ab][:], in1=dsta, op=ALU.add), reads=[abk, ('Eacc', sl, half)], writes=[('Eacc', sl, half)])
                T.op('sp', lambda e: e.dma_start(out=ybuf[ex * cap:(ex + 1) * cap, :].rearrange("(s p) f -> p s f", p=128), in_=acc[:]),
                     reads=[('Eacc', sl, half) for sl in range(cap // 128) for half in range(2)], writes=[('ybuf', ex)], dsem='ybw')
            T.barrier()
        with contextlib.ExitStack() as es:
            def sb(name, shape, dtp):
                return es.enter_context(nc.sbuf_tensor(_PFX[0] + name, shape, dtp))
            y1 = [sb("Cy1_%d" % i, [128, D], F32) for i in range(2)]
            y2 = [sb("Cy2_%d" % i, [128, D], F32) for i in range(2)]
            xt = [sb("Cxt%d" % i, [128, D], F32) for i in range(2)]
            xo = [sb("Cxo%d" % i, [128, D], F32) for i in range(2)]
            if last:
                fg_ = sb("fgrow", [1, D], F32); FG = sb("FG", [128, D], F32)
                junk = sb("Cjunk", [128, D], BF16); ss = [sb("Css%d" % i, [128, 2], F32) for i in range(2)]
                nhalf = sb("Cnhalf", [128, 1], F32)
                T.op('pool', lambda e: e.memset(nhalf[:], -0.5), writes=['Cnhalf'])
                T.op('sp', lambda e: e.dma_start(out=fg_[:], in_=fng), writes=['fgrow'], dsem='c0')
                for half in range(2):
                    T.op('pe', lambda e: e.matmul(ps[half][:], lhsT=ones[0:1, :], rhs=fg_[0:1, half * 512:(half + 1) * 512], start=True, stop=True), reads=['ones', 'fgrow'], writes=[('ps', half)])
                    T.op('dve', lambda e: e.tensor_copy(out=FG[:, half * 512:(half + 1) * 512], in_=ps[half][:]), reads=[('ps', half)], writes=[('FG', half)])
            for t in range(ntiles):
                which = 0 if t < 32 else 1
                o = t % 2
                T.op('sp', lambda e: e.dma_start(out=xt[o][:], in_=X1d[t * 128:(t + 1) * 128, :]), writes=[('Cxt', o)], dsem='Cxt%d' % o)
                T.op('pool', lambda e: e.memset(y1[o][:], 0.0), writes=[('Cy1', o)])
                T.op('pool', lambda e: e.memset(y2[o][:], 0.0), writes=[('Cy2', o)])
                T.op('pool', lambda e: e.indirect_dma_start(out=y1[o][:, :], out_offset=None, in_=ybuf[:, :], in_offset=bass.IndirectOffsetOnAxis(ap=dest[:, 2 * t:2 * t + 1], axis=0),
                                                            bounds_check=bc_reg, oob_is_err=False), reads=['ybuf_all'], writes=[('Cy1', o)], dsem='Cy1_%d' % o)
                T.op('pool', lambda e: e.indirect_dma_start(out=y2[o][:, :], out_offset=None, in_=ybuf[:, :], in_offset=bass.IndirectOffsetOnAxis(ap=dest[:, 2 * t + 1:2 * t + 2], axis=0),
                                                            bounds_check=bc_reg, oob_is_err=False), reads=['ybuf_all'], writes=[('Cy2', o)], dsem='Cy2_%d' % o)
                T.op('dve', lambda e: e.tensor_scalar(out=y1[o][:], in0=y1[o][:], scalar1=gate[:, 2 * t:2 * t + 1], scalar2=None, op0=ALU.mult), reads=[('Cy1', o)], writes=[('Cy1', o)])
                T.op('dve', lambda e: e.scalar_tensor_tensor(out=y1[o][:], in0=y2[o][:], scalar=gate[:, 2 * t + 1:2 * t + 2], in1=y1[o][:], op0=ALU.mult, op1=ALU.add), reads=[('Cy1', o), ('Cy2', o)], writes=[('Cy1', o)])
                T.op('pool', lambda e: e.tensor_tensor(out=y1[o][:], in0=y1[o][:], in1=mods[(which, 5)][:], op=ALU.mult), reads=[('Cy1', o), ('mod', which, 5)], writes=[('Cy1', o)])
                T.op('pool', lambda e: e.tensor_tensor(out=xo[o][:], in0=y1[o][:], in1=xt[o][:], op=ALU.add), reads=[('Cy1', o), ('Cxt', o)], writes=[('Cxo', o)])
                if last:
                    T.op('act', lambda e: e.activation(out=junk[:], in_=xo[o][:], func=AF.Square, accum_out=ss[o][:, 0:1]), reads=[('Cxo', o)], writes=[('Css', o)])
                    T.op('dve', lambda e: e.tensor_scalar(out=ss[o][:, 1:2], in0=ss[o][:, 0:1], scalar1=1.0 / D, scalar2=EPS, op0=ALU.mult, op1=ALU.add), reads=[('Css', o)], writes=[('Css1', o)])
                    T.op('pool', lambda e: e.tensor_tensor(out=ss[o][:, 1:2], in0=ss[o][:, 1:2], in1=nhalf[:], op=ALU.pow), reads=[('Css1', o), 'Cnhalf'], writes=[('Css1', o)])
                    T.op('dve', lambda e: e.scalar_tensor_tensor(out=xo[o][:], in0=xo[o][:], scalar=ss[o][:, 1:2], in1=FG[:], op0=ALU.mult, op1=ALU.mult),
                         reads=[('Cxo', o), ('Css1', o), ('FG', 0), ('FG', 1)], writes=[('Cxo', o)])
                dst = yown[t * 128:(t + 1) * 128, :] if t < 32 else yc[(t - 32) * 128:(t - 31) * 128, :]
                T.op('sp', lambda e: e.dma_start(out=dst, in_=xo[o][:]), reads=[('Cxo', o)], writes=[('yo', t)], dsem='yout')


def attn_host_inputs(inp, i, x_lat, x_ctx, core, last, cap=CAP):
    j = i // 2
    b = core // 2; s = core % 2
    xw = np.zeros((NW * 128, D), np.float32)
    lo = s * 4096 - 128; hi = s * 4096 + 4096 + 128
    a = max(lo, 0); bb = min(hi, S)
    xw[a - lo: bb - lo] = x_lat[b, a:bb]
    wqkv = inp['attn_w_qkv'][j]
    wq = wqkv[:, :1024]; wk = wqkv[:, 1024:1280]; wv = wqkv[:, 1280:1536]
    d = np.arange(64)
    partner = np.where((d % 32) < 16, d + 16, d - 16)
    permq = (np.arange(16)[:, None] * 64 + partner[None, :]).reshape(-1)
    permk = (np.arange(4)[:, None] * 64 + partner[None, :]).reshape(-1)
    wqr = wq[:, permq]; wkr = wk[:, permk]
    wk2 = np.concatenate([wk.reshape(D, 4, 1, 64)] * 2, axis=2).reshape(D, 512)
    wk2r = np.concatenate([wkr.reshape(D, 4, 1, 64)] * 2, axis=2).reshape(D, 512)
    w = np.arange(NW * 128); t = s * 4096 - 128 + w
    valid = (t >= 0) & (t < S)
    tt = np.where(valid, t, 0)
    row = (tt // 64).astype(np.float32); col = (tt % 64).astype(np.float32)
    inv = (10000.0 ** (-np.arange(16, dtype=np.float32) / 16)).astype(np.float32)
    axis = d // 32; half = (d % 32) // 16; f = d % 16
    pos = np.where(axis[:, None] == 0, row[None, :], col[None, :]).astype(np.float32)
    ang = (pos * inv[f][:, None]).astype(np.float32)
    cosv = np.cos(ang).astype(np.float32); sinv = np.sin(ang).astype(np.float32)
    sins = np.where(half[:, None] == 0, -sinv, sinv)
    cost = np.ones((128, NTOK), np.float32); sint = np.zeros((128, NTOK), np.float32)
    cost[:, :NW * 128] = np.concatenate([cosv, cosv], 0); sint[:, :NW * 128] = np.concatenate([sins, sins], 0)
    jj = np.arange(128)[:, None]; qq = np.arange(128)[None, :]
    mP = (jj >= qq).astype(np.float32); mN = (jj <= qq).astype(np.float32)
    vP = 1.0 if s == 1 else 0.0; vN = 1.0 if s == 0 else 0.0
    masks = np.concatenate([mP, mP * vP, mN, mN * vN], axis=1).astype(np.float32)
    cvec = np.concatenate([inp['c'][b].reshape(8, 128).T, inp['c_ctx'].reshape(8, 128).T], axis=1).astype(np.float32)
    return dict(xwin=xw, xc=np.ascontiguousarray(x_ctx[b]), cvec=np.ascontiguousarray(cvec), ada_w=inp['ada_w'][i], ada_b=inp['ada_b'][i][None, :],
                nmg=inp['norm_mix_g'][i][None, :], nfg=inp['norm_ffn_g'][i][None, :],
                wq=np.ascontiguousarray(wq), wqr=np.ascontiguousarray(wqr), wk2=np.ascontiguousarray(wk2), wk2r=np.ascontiguousarray(wk2r),
                wv=np.ascontiguousarray(wv), wo=inp['attn_w_o'][j], cost=cost, sint=sint, sinks=inp['attn_sinks'][j][None, :], masks=masks,
                wr=inp['moe_w_router'][j], mwg=inp['moe_w_gate'][j], mwu=inp['moe_w_up'][j], mwd=inp['moe_w_down'][j],
                fng=inp['final_norm_g'][None, :], ident=np.eye(128, dtype=np.float32),
                ut=np.triu(np.ones((128, 128), np.float32), 1), ebase=np.tile((np.arange(8) * cap).astype(np.float32)[None, :], (128, 1)))


_FNET_W = ['cvec', 'ada_w', 'ada_b', 'nmg', 'nfg', 'w_out', 'wg', 'wu', 'wd']
_ATTN_W = ['cvec', 'ada_w', 'ada_b', 'nmg', 'nfg', 'wq', 'wqr', 'wk2', 'wk2r', 'wv', 'wo', 'sinks', 'wr', 'mwg', 'mwu', 'mwd']
_SHAPES = dict(cvec=[128, 16], ada_w=[D, 6 * D], ada_b=[1, 6 * D], nmg=[1, D], nfg=[1, D], w_out=[D, D], wg=[D, DFF], wu=[D, DFF], wd=[DFF, D],
               wq=[D, D], wqr=[D, D], wk2=[D, 512], wk2r=[D, 512], wv=[D, 256], wo=[D, D], sinks=[1, 16], wr=[D, 8],
               mwg=[8, D, DFF], mwu=[8, D, DFF], mwd=[8, DFF, D])
_SHARED = dict(cs256=[256, 512], twc=[128, 64 * 128], tws=[128, 64 * 128], w64_0=[128, 32], w64_1=[128, 32],
               cost_0=[128, NTOK], sint_0=[128, NTOK], masks_0=[128, 512], cost_1=[128, NTOK], sint_1=[128, NTOK], masks_1=[128, 512],
               fng=[1, D], ut=[128, 128], ebase=[128, 8])


def build_fused(cap=CAP):
    nc = bass.Bass("TRN2", target_bir_lowering=False)

    def dt(name, shape, dtype=F32, kind="ExternalInput"):
        return nc.dram_tensor(name, shape, dtype, kind=kind).ap()
    x = dt("x", [S, D]); ctx = dt("ctx", [256, D]); ident = dt("ident", [128, 128])
    out = dt("out", [4096, D], kind="ExternalOutput")
    shared = {k: dt(k, shp) for k, shp in _SHARED.items()}
    LW = []
    for i in range(4):
        names = _FNET_W if i % 2 == 0 else _ATTN_W
        w = {k: dt("L%d_%s" % (i, k), _SHAPES[k]) for k in names}
        w.update(shared)
        LW.append(w)
    Y = [dt("Y%d" % i, [S, D], F32, kind="Internal") for i in range(3)]
    C = [dt("C%d" % i, [256, D], F32, kind="Internal") for i in range(3)]
    SC = dict(ABd=dt("ABd", [S, 2048], BF16, kind="Internal"), Gd=dt("Gd", [64, 128, 2, 1024], BF16, kind="Internal"),
              X1d=dt("X1d", [4352, D], F32, kind="Internal"),
              wgb=dt("wgb", [D, DFF], BF16, kind="Internal"), wub=dt("wub", [D, DFF], BF16, kind="Internal"),
              wdb=dt("wdb", [DFF, D], BF16, kind="Internal"), woutb=dt("woutb", [D, D], BF16, kind="Internal"),
              hTd=dt("hTd", [128, 8, NTOK], BF16, kind="Internal"), qTd=dt("qTd", [128, 8, 4352], BF16, kind="Internal"),
              modc=dt("modc", [12, 128, D], F32, kind="Internal"), hbuf=dt("hbuf", [8 * cap, D], BF16, kind="Internal"), ybuf=dt("ybuf", [8 * cap, D], F32, kind="Internal"))
    with contextlib.ExitStack() as es:
        T = Tr(nc, es)

        def sbg(name, shape, dtp):
            return es.enter_context(nc.sbuf_tensor(name, shape, dtp))
        ps = [es.enter_context(nc.psum_tensor("bank%d" % i, [128, 512], F32)) for i in range(8)]
        common = setup_common(nc, T, es, sbg, ps, ident)
        T.barrier()
        for i in range(4):
            A = dict(xin=x if i == 0 else Y[i - 1], cin=ctx if i == 0 else C[i - 1],
                     yout=out if i == 3 else Y[i], cout=None if i == 3 else C[i])
            for s in range(1 if i == 3 else 2):
                _PFX[0] = "L%ds%d_" % (i, s)
                if i % 2 == 0:
                    emit_fnet(nc, T, ps, common, LW[i], A, SC, s, first_pass=(s == 0), lat_tiles=([0, 31] if (i == 2 and s == 1) else None))
                else:
                    emit_attn(nc, T, ps, common, LW[i], A, SC, s, do_ctx=(s == 0 and i != 3), final=(i == 3), cap=cap, mod_load=(s == 1))
                T.barrier()
    return nc


def attn_tables(s):
    d = np.arange(64)
    w = np.arange(NW * 128); t = s * 4096 - 128 + w
    valid = (t >= 0) & (t < S)
    tt = np.where(valid, t, 0)
    row = (tt // 64).astype(np.float32); col = (tt % 64).astype(np.float32)
    inv = (10000.0 ** (-np.arange(16, dtype=np.float32) / 16)).astype(np.float32)
    axis = d // 32; half = (d % 32) // 16; f = d % 16
    pos = np.where(axis[:, None] == 0, row[None, :], col[None, :]).astype(np.float32)
    ang = (pos * inv[f][:, None]).astype(np.float32)
    cosv = np.cos(ang).astype(np.float32); sinv = np.sin(ang).astype(np.float32)
    sins = np.where(half[:, None] == 0, -sinv, sinv)
    cost = np.ones((128, NTOK), np.float32); sint = np.zeros((128, NTOK), np.float32)
    cost[:, :NW * 128] = np.concatenate([cosv, cosv], 0); sint[:, :NW * 128] = np.concatenate([sins, sins], 0)
    jj = np.arange(128)[:, None]; qq = np.arange(128)[None, :]
    mP = (jj >= qq).astype(np.float32); mN = (jj <= qq).astype(np.float32)
    vP = 1.0 if s == 1 else 0.0; vN = 1.0 if s == 0 else 0.0
    masks = np.concatenate([mP, mP * vP, mN, mN * vN], axis=1).astype(np.float32)
    return cost, sint, masks


def host_inputs(inp, b, swapped, cap=CAP):
    xb = inp['x'][b]
    if swapped:
        xb = np.concatenate([xb[4096:], xb[:4096]], axis=0)
    m = dict(x=np.ascontiguousarray(xb), ctx=np.ascontiguousarray(inp['ctx'][b]), ident=np.eye(128, dtype=np.float32))
    rs = (lambda s: 1 - s) if swapped else (lambda s: s)
    cs256, twc, tws, w64_0 = fnet_tables(rs(0), shift=4096 if swapped else 0)
    _, _, _, w64_1 = fnet_tables(rs(1), shift=4096 if swapped else 0)
    m.update(cs256=cs256, twc=twc, tws=tws, w64_0=w64_0, w64_1=w64_1)
    for s in range(2):
        cost, sint, masks = attn_tables(rs(s))
        m['cost_%d' % s] = cost; m['sint_%d' % s] = sint; m['masks_%d' % s] = masks
    m['fng'] = inp['final_norm_g'][None, :]
    m['ut'] = np.triu(np.ones((128, 128), np.float32), 1)
    m['ebase'] = np.tile((np.arange(8) * cap).astype(np.float32)[None, :], (128, 1))
    cvec = np.ascontiguousarray(np.concatenate([inp['c'][b].reshape(8, 128).T, inp['c_ctx'].reshape(8, 128).T], axis=1).astype(np.float32))
    d = np.arange(64)
    partner = np.where((d % 32) < 16, d + 16, d - 16)
    permq = (np.arange(16)[:, None] * 64 + partner[None, :]).reshape(-1)
    permk = (np.arange(4)[:, None] * 64 + partner[None, :]).reshape(-1)
    for i in range(4):
        j = i // 2
        w = dict(cvec=cvec, ada_w=inp['ada_w'][i], ada_b=inp['ada_b'][i][None, :], nmg=inp['norm_mix_g'][i][None, :], nfg=inp['norm_ffn_g'][i][None, :])
        if i % 2 == 0:
            w.update(w_out=inp['fnet_w_out'][j], wg=inp['ffn_w_gate'][j], wu=inp['ffn_w_up'][j], wd=inp['ffn_w_down'][j])
        else:
            wqkv = inp['attn_w_qkv'][j]
            wq = wqkv[:, :1024]; wk = wqkv[:, 1024:1280]; wv = wqkv[:, 1280:1536]
            wqr = wq[:, permq]; wkr = wk[:, permk]
            wk2 = np.concatenate([wk.reshape(D, 4, 1, 64)] * 2, axis=2).reshape(D, 512)
            wk2r = np.concatenate([wkr.reshape(D, 4, 1, 64)] * 2, axis=2).reshape(D, 512)
            w.update(wq=wq, wqr=wqr, wk2=wk2, wk2r=wk2r, wv=wv, wo=inp['attn_w_o'][j], sinks=inp['attn_sinks'][j][None, :],
                     wr=inp['moe_w_router'][j], mwg=inp['moe_w_gate'][j], mwu=inp['moe_w_up'][j], mwd=inp['moe_w_down'][j])
        for k, v in w.items():
            m['L%d_%s' % (i, k)] = np.ascontiguousarray(v, dtype=np.float32)
    return m


_PROG = {}


def kernel(**inputs):
    inp = {k: np.ascontiguousarray(np.asarray(v, dtype=np.float32)) for k, v in inputs.items()}
    if 'nc' not in _PROG:
        _PROG['nc'] = build_fused()
    nc = _PROG['nc']
    in_maps = [host_inputs(inp, c % 4, swapped=(c >= 4)) for c in range(8)]
    res = run_bass_kernel_spmd(nc, in_maps, core_ids=list(range(8)))
    return np.ascontiguousarray(np.stack([np.concatenate([res.results[b]['out'], res.results[b + 4]['out']], axis=0) for b in range(4)]).astype(np.float32))
```
